# Optimizing a Trainium2 kernel written in Bass

```python
import math
import jax, jax.numpy as jnp
from jax import lax
import numpy as np

D_MODEL = 1024
BATCH = 8
SEQ = 4096
DEPTH = 1

N_META = 16
CHUNK = 128
ROPE_BASE = 10000.0
EPS = 1e-6
MLA_HEADS = 8
MLA_Q_LORA = 384
MLA_KV_LORA = 256
MLA_NOPE = 64
MLA_ROPE = 32
MLA_V = 64
MLA_QK = MLA_NOPE + MLA_ROPE
RET_HEADS = 8
RET_DK = 64
RET_DV = 128
PEER_HEADS = 8
PEER_NKEYS = 128
PEER_EXPERTS = PEER_NKEYS * PEER_NKEYS
PEER_DQ = 128
PEER_TOPK = 16
PEER_BLOCK = 256
IN_SIZES = (MLA_Q_LORA, MLA_KV_LORA, MLA_ROPE, RET_HEADS * RET_DK, RET_HEADS * RET_DK,
            RET_HEADS * RET_DV, RET_HEADS * RET_DV, D_MODEL, D_MODEL)
IN_WIDTH = sum(IN_SIZES)

kernel_name = "hybrid_mla_retention_peer_meta"


def rmsnorm(x, g):
    xf = x.astype(jnp.float32)
    y = xf * lax.rsqrt(jnp.mean(xf * xf, axis=-1, keepdims=True) + EPS)
    return (y * g.astype(jnp.float32)).astype(x.dtype)


def rope(x, pos):
    half = x.shape[-1] // 2
    inv = ROPE_BASE ** (-jnp.arange(half, dtype=jnp.float32) / half)
    ang = pos.astype(jnp.float32)[:, None] * inv[None, :]
    cos, sin = jnp.cos(ang), jnp.sin(ang)
    xf = x.astype(jnp.float32)
    x1, x2 = xf[..., :half], xf[..., half:]
    return jnp.concatenate([x1 * cos - x2 * sin, x1 * sin + x2 * cos], -1).astype(x.dtype)


def causal_block_attention(q, k, v):
    B, H, L, dqk = q.shape
    dv = v.shape[-1]
    nblk = -(-L // CHUNK)
    Lp = nblk * CHUNK
    qp = jnp.pad(q, ((0, 0), (0, 0), (0, Lp - L), (0, 0)))
    kpos = jnp.arange(L)
    scale = 1.0 / math.sqrt(dqk)

    def one_block(i):
        qb = lax.dynamic_slice_in_dim(qp, i * CHUNK, CHUNK, axis=2)
        s = jnp.einsum('bhqd,bhkd->bhqk', qb, k).astype(jnp.float32) * scale
        qpos = i * CHUNK + jnp.arange(CHUNK)
        s = jnp.where(kpos[None, :] <= qpos[:, None], s, -jnp.inf)
        p = jax.nn.softmax(s, axis=-1)
        return jnp.einsum('bhqk,bhkd->bhqd', p.astype(v.dtype), v)

    out = lax.map(one_block, jnp.arange(nblk))
    out = out.transpose(1, 2, 0, 3, 4).reshape(B, H, Lp, dv)
    return out[:, :, :L]


def mla_branch(c_q, c_kv, k_pe, pos, g_q_lora, w_uq, g_kv_lora, w_ukv, g_qk_q, g_qk_k):
    B, L, _ = c_q.shape
    q = (rmsnorm(c_q, g_q_lora) @ w_uq).reshape(B, L, MLA_HEADS, MLA_QK)
    kv = (rmsnorm(c_kv, g_kv_lora) @ w_ukv).reshape(B, L, MLA_HEADS, MLA_NOPE + MLA_V)
    k_nope, v = kv[..., :MLA_NOPE], kv[..., MLA_NOPE:]
    k = jnp.concatenate([k_nope, jnp.broadcast_to(k_pe[:, :, None, :], (B, L, MLA_HEADS, MLA_ROPE))], -1)
    q = rmsnorm(q, g_qk_q).transpose(0, 2, 1, 3)
    k = rmsnorm(k, g_qk_k).transpose(0, 2, 1, 3)
    q = jnp.concatenate([q[..., :MLA_NOPE], rope(q[..., MLA_NOPE:], pos)], -1)
    k = jnp.concatenate([k[..., :MLA_NOPE], rope(k[..., MLA_NOPE:], pos)], -1)
    o = causal_block_attention(q, k, v.transpose(0, 2, 1, 3))
    return o.transpose(0, 2, 1, 3).reshape(B, L, MLA_HEADS * MLA_V)


def retention_branch(q, k, v, gate, pos, g_gn):
    B, L, _ = q.shape
    H, C = RET_HEADS, CHUNK
    q = rope(q.reshape(B, L, H, RET_DK).transpose(0, 2, 1, 3), pos)
    k = rope(k.reshape(B, L, H, RET_DK).transpose(0, 2, 1, 3), pos) * (RET_DK ** -0.5)
    v = v.reshape(B, L, H, RET_DV).transpose(0, 2, 1, 3)
    P = (-N_META) % C
    Lp = L + P
    NC = Lp // C
    padf = lambda t: jnp.pad(t, ((0, 0), (0, 0), (P, 0), (0, 0))).reshape(B, H, NC, C, t.shape[-1])
    qc, kc, vc = padf(q), padf(k), padf(v)

    log_gamma = jnp.log(1.0 - 2.0 ** (-5.0 - jnp.arange(H, dtype=jnp.float32)))
    idx = jnp.arange(C, dtype=jnp.float32)
    diff = idx[:, None] - idx[None, :]
    decay = jnp.where(diff[None] >= 0, jnp.exp(jnp.maximum(diff, 0.0)[None] * log_gamma[:, None, None]), 0.0)
    zeta = jnp.exp((C - 1 - idx)[None, :] * log_gamma[:, None])
    xi = jnp.exp((idx + 1)[None, :] * log_gamma[:, None])
    gamma_c = jnp.exp(C * log_gamma)

    scores = jnp.einsum('bhncd,bhnmd->bhncm', qc, kc) * decay[None, :, None]
    inner = jnp.einsum('bhncm,bhnme->bhnce', scores, vc)
    chunk_kv = jnp.einsum('bhnmd,bhnme->bhnde', kc * zeta[None, :, None, :, None], vc)

    def step(state, kv_n):
        return gamma_c[None, :, None, None] * state + kv_n, state

    init = jnp.zeros((B, H, RET_DK, RET_DV), chunk_kv.dtype)
    _, prev_states = lax.scan(step, init, jnp.moveaxis(chunk_kv, 2, 0))
    prev_states = jnp.moveaxis(prev_states, 0, 2)
    cross = jnp.einsum('bhncd,bhnde->bhnce', qc, prev_states) * xi[None, :, None, :, None]
    y = (inner + cross).reshape(B, H, Lp, RET_DV)[:, :, P:]

    yf = y.astype(jnp.float32)
    mu = jnp.mean(yf, -1, keepdims=True)
    var = jnp.mean((yf - mu) ** 2, -1, keepdims=True)
    yn = ((yf - mu) * lax.rsqrt(var + EPS)).transpose(0, 2, 1, 3).reshape(B, L, H * RET_DV)
    yn = (yn * g_gn.astype(jnp.float32)).astype(gate.dtype)
    return jax.nn.silu(gate) * yn


def mixer_sublayer(h, pos, g_mix, w_in, g_q_lora, w_uq, g_kv_lora, w_ukv, g_qk_q, g_qk_k,
                   w_mla_out, g_ret_gn, w_ret_out, w_mix_out):
    hn = rmsnorm(h, g_mix)
    z = hn @ w_in
    c_q, c_kv, k_pe, r_q, r_k, r_v, r_g, za, zr = jnp.split(z, np.cumsum(IN_SIZES)[:-1].tolist(), axis=-1)
    y_a = mla_branch(c_q, c_kv, k_pe, pos, g_q_lora, w_uq, g_kv_lora, w_ukv, g_qk_q, g_qk_k) @ w_mla_out
    y_r = retention_branch(r_q, r_k, r_v, r_g, pos, g_ret_gn) @ w_ret_out
    merged = jax.nn.sigmoid(za) * y_a + jax.nn.sigmoid(zr) * y_r
    return merged @ w_mix_out


def peer_sublayer(h, g_ffn, w_peer_q, keys_1, keys_2, peer_u, peer_v):
    shape = h.shape
    x = rmsnorm(h, g_ffn).reshape(-1, shape[-1])
    T = x.shape[0]
    q = (x @ w_peer_q).reshape(T, PEER_HEADS, PEER_DQ)
    half = PEER_DQ // 2
    s1 = jnp.einsum('thd,kd->thk', q[..., :half], keys_1).astype(jnp.float32)
    s2 = jnp.einsum('thd,kd->thk', q[..., half:], keys_2).astype(jnp.float32)
    v1, i1 = lax.top_k(s1, PEER_TOPK)
    v2, i2 = lax.top_k(s2, PEER_TOPK)
    cand = (v1[..., :, None] + v2[..., None, :]).reshape(T, PEER_HEADS, PEER_TOPK * PEER_TOPK)
    sc, ci = lax.top_k(cand, PEER_TOPK)
    e1 = jnp.take_along_axis(i1, ci // PEER_TOPK, axis=-1)
    e2 = jnp.take_along_axis(i2, ci % PEER_TOPK, axis=-1)
    eidx = (e1 * PEER_NKEYS + e2).reshape(T, PEER_HEADS * PEER_TOPK)
    gates = jax.nn.softmax(sc, axis=-1).reshape(T, PEER_HEADS * PEER_TOPK).astype(x.dtype)

    pad = (-T) % PEER_BLOCK
    nb = (T + pad) // PEER_BLOCK
    xb = jnp.pad(x, ((0, pad), (0, 0))).reshape(nb, PEER_BLOCK, -1)
    ib = jnp.pad(eidx, ((0, pad), (0, 0))).reshape(nb, PEER_BLOCK, -1)
    gb = jnp.pad(gates, ((0, pad), (0, 0))).reshape(nb, PEER_BLOCK, -1)

    def block(args):
        xt, it, gt = args
        act = jax.nn.gelu(jnp.einsum('td,tkd->tk', xt, peer_u[it]), approximate=False)
        return jnp.einsum('tk,tkd->td', gt * act, peer_v[it])

    y = lax.map(block, (xb, ib, gb)).reshape(nb * PEER_BLOCK, -1)[:T]
    return y.reshape(shape)


def setup_inputs(seed: int = 0) -> dict:
    key = jax.random.key(seed)
    ks = jax.random.split(key, 20)
    f32 = jnp.float32
    nrm = lambda k, s, sc: jax.random.normal(k, s, f32) * sc
    gain = lambda k, s: 1.0 + 0.02 * jax.random.normal(k, s, f32)
    return {
        "x": nrm(ks[0], (BATCH, SEQ, D_MODEL), 1.0),
        "meta_tokens": nrm(ks[1], (N_META, D_MODEL), 1.0),
        "g_mix": gain(ks[2], (DEPTH, D_MODEL)),
        "w_in": nrm(ks[3], (DEPTH, D_MODEL, IN_WIDTH), D_MODEL ** -0.5),
        "g_q_lora": gain(ks[4], (DEPTH, MLA_Q_LORA)),
        "w_uq": nrm(ks[5], (DEPTH, MLA_Q_LORA, MLA_HEADS * MLA_QK), MLA_Q_LORA ** -0.5),
        "g_kv_lora": gain(ks[6], (DEPTH, MLA_KV_LORA)),
        "w_ukv": nrm(ks[7], (DEPTH, MLA_KV_LORA, MLA_HEADS * (MLA_NOPE + MLA_V)), MLA_KV_LORA ** -0.5),
        "g_qk_q": gain(ks[8], (DEPTH, MLA_QK)),
        "g_qk_k": gain(ks[9], (DEPTH, MLA_QK)),
        "w_mla_out": nrm(ks[10], (DEPTH, MLA_HEADS * MLA_V, D_MODEL), (MLA_HEADS * MLA_V) ** -0.5),
        "g_ret_gn": gain(ks[11], (DEPTH, RET_HEADS * RET_DV)),
        "w_ret_out": nrm(ks[12], (DEPTH, RET_HEADS * RET_DV, D_MODEL), (RET_HEADS * RET_DV) ** -0.5),
        "w_mix_out": nrm(ks[13], (DEPTH, D_MODEL, D_MODEL), D_MODEL ** -0.5),
        "g_ffn": gain(ks[14], (DEPTH, D_MODEL)),
        "w_peer_q": nrm(ks[15], (DEPTH, D_MODEL, PEER_HEADS * PEER_DQ), D_MODEL ** -0.5),
        "peer_keys_1": nrm(ks[16], (DEPTH, PEER_NKEYS, PEER_DQ // 2), (PEER_DQ // 2) ** -0.5),
        "peer_keys_2": nrm(ks[17], (DEPTH, PEER_NKEYS, PEER_DQ // 2), (PEER_DQ // 2) ** -0.5),
        "peer_u": nrm(ks[18], (DEPTH, PEER_EXPERTS, D_MODEL), D_MODEL ** -0.5),
        "peer_v": nrm(ks[19], (DEPTH, PEER_EXPERTS, D_MODEL), PEER_HEADS ** -0.5),
    }


def reference(x, meta_tokens, g_mix, w_in, g_q_lora, w_uq, g_kv_lora, w_ukv, g_qk_q, g_qk_k,
              w_mla_out, g_ret_gn, w_ret_out, w_mix_out, g_ffn, w_peer_q, peer_keys_1,
              peer_keys_2, peer_u, peer_v):
    B = x.shape[0]
    meta = jnp.broadcast_to(meta_tokens.astype(x.dtype)[None], (B, N_META, x.shape[-1]))
    h = jnp.concatenate([meta, x], axis=1)
    pos = jnp.arange(h.shape[1])
    for layer in range(DEPTH):
        h = h + mixer_sublayer(h, pos, g_mix[layer], w_in[layer], g_q_lora[layer], w_uq[layer],
                               g_kv_lora[layer], w_ukv[layer], g_qk_q[layer], g_qk_k[layer],
                               w_mla_out[layer], g_ret_gn[layer], w_ret_out[layer], w_mix_out[layer])
        if layer == DEPTH - 1:
            h = h[:, N_META:]
        h = h + peer_sublayer(h, g_ffn[layer], w_peer_q[layer], peer_keys_1[layer], peer_keys_2[layer],
                              peer_u[layer], peer_v[layer])
    return h
```

```python
import math
import numpy as np
from contextlib import ExitStack
import concourse.bass as bass
import concourse.mybir as mybir
from concourse.bass_utils import run_bass_kernel_spmd

F32 = mybir.dt.float32
BF16 = mybir.dt.bfloat16
I32 = mybir.dt.int32
U32 = mybir.dt.uint32
AF = mybir.ActivationFunctionType
ALU = mybir.AluOpType
AX = mybir.AxisListType

NT = 33
LP = NT * 128
EPS = 1e-6
NEG = -1.0e30
GROUPS = [(0, 384), (384, 672), (672, 1184), (1184, 1696), (1696, 2208), (2208, 2720),
          (2720, 3232), (3232, 3744), (3744, 4256), (4256, 4768), (4768, 5280), (5280, 5792)]


class Tl:
    def __init__(self, t):
        self.t = t
        self.w = {}
        self.r = {}

    def __getitem__(self, k):
        return self.t[k]


class KB:
    def __init__(self):
        nc = bass.Bass("TRN2", target_bir_lowering=False)
        self.nc = nc
        self.es = ExitStack()
        self.E = dict(pe=nc.tensor, dve=nc.vector, act=nc.scalar, pool=nc.gpsimd, sp=nc.sync)
        self.csem = {e: self.es.enter_context(nc.semaphore("c_" + e)) for e in ("pe", "dve", "act", "pool")}
        self.ccnt = {e: 0 for e in self.csem}
        self.NDS = 8
        self.dsem = {q: [self.es.enter_context(nc.semaphore("d_%s%d" % (q, i))) for i in range(self.NDS)]
                     for q in ("sp", "pool")}
        self.dcnt = {q: [0] * self.NDS for q in ("sp", "pool")}
        self.dnext = {q: 0 for q in ("sp", "pool")}
        self.waited = {e: {} for e in self.E}
        self.nwaits = 0
        self.ninst = 0

    def sb(self, es, name, shape, dt):
        return Tl(es.enter_context(self.nc.sbuf_tensor("s_" + name, shape, dt)))

    def ps(self, es, name, shape, dt):
        return Tl(es.enter_context(self.nc.psum_tensor("p_" + name, shape, dt)))

    def dram(self, name, shape, dt, kind):
        return Tl(self.nc.dram_tensor(name, shape, dt, kind=kind).ap())

    def _deps(self, r, w, pw):
        deps = []
        for b in r:
            deps.extend(b.w.values())
        for b in w:
            deps.extend(b.w.values())
            deps.extend(b.r.values())
        for b in pw:
            deps.extend(b.r.values())
        return deps

    def _wait(self, e, deps):
        best = {}
        for (key, sem, val, src) in deps:
            if src == e and e == "pe":
                continue
            if key not in best or best[key][1] < val:
                best[key] = (sem, val)
        for key, (sem, val) in best.items():
            if self.waited[e].get(key, 0) >= val:
                continue
            self.E[e].wait_ge(sem, val)
            self.waited[e][key] = val
            self.nwaits += 1

    def _commit(self, ev, r, w, pw):
        key = ev[0]
        for b in w:
            b.w = {key: ev}
            b.r = {}
        for b in pw:
            b.w[key] = ev
        for b in r:
            if not any(b is x for x in w) and not any(b is x for x in pw):
                b.r[key] = ev

    def op(self, e, fn, r=(), w=(), pw=()):
        self._wait(e, self._deps(r, w, pw))
        inst = fn(self.E[e])
        self.ccnt[e] += 1
        inst.then_inc(self.csem[e], 1)
        ev = ("c_" + e, self.csem[e], self.ccnt[e], e)
        self._commit(ev, r, w, pw)
        self.ninst += 1
        return ev

    def dma(self, q, out, in_, r=(), w=(), pw=(), fn=None):
        deps = self._deps(r, w, pw)
        i = self.dnext[q]
        self.dnext[q] = (i + 1) % self.NDS
        sem = self.dsem[q][i]
        key = "d_%s%d" % (q, i)
        if self.dcnt[q][i] > 0:
            deps.append((key, sem, self.dcnt[q][i], "dma"))
        self._wait(q, deps)
        if fn is None:
            inst = self.E[q].dma_start(out=out, in_=in_)
        else:
            inst = fn(self.E[q])
        self.dcnt[q][i] += 16
        inst.then_inc(sem, 16)
        ev = (key, sem, self.dcnt[q][i], "dma")
        self._commit(ev, r, w, pw)
        self.ninst += 1
        return ev

    def all_events(self):
        evs = []
        for e in self.csem:
            if self.ccnt[e] > 0:
                evs.append(("c_" + e, self.csem[e], self.ccnt[e], e))
        for q in self.dsem:
            for i in range(self.NDS):
                if self.dcnt[q][i] > 0:
                    evs.append(("d_%s%d" % (q, i), self.dsem[q][i], self.dcnt[q][i], "dma"))
        return evs

    def barrier(self):
        evs = self.all_events()
        for e in self.E:
            self._wait(e, [ev for ev in evs if not (ev[3] == e and e == "pe")])


def tt(kb, e, out, a, b, op, r, w, pw=()):
    return kb.op(e, lambda g: g.tensor_tensor(out=out, in0=a, in1=b, op=op), r=r, w=w, pw=pw)


def rope(kb, e, o1, o2, x1, x2, cos, sin, ta, tb, rd, wr):
    A, B = ta[1], tb[1]
    tt(kb, e, A, x1, cos, ALU.mult, rd, [ta[0]])
    tt(kb, e, B, x2, sin, ALU.mult, rd, [tb[0]])
    tt(kb, e, o1, A, B, ALU.subtract, [ta[0], tb[0]], [], pw=wr)
    tt(kb, e, A, x1, sin, ALU.mult, rd, [ta[0]])
    tt(kb, e, B, x2, cos, ALU.mult, rd, [tb[0]])
    tt(kb, e, o2, A, B, ALU.add, [ta[0], tb[0]], [], pw=wr)


def rstd_from_ss(kb, ss, rs, epsc, n):
    kb.op("act", lambda g: g.activation(out=rs[0], in_=ss[0], func=AF.Sqrt, bias=epsc[:, 0:1], scale=1.0 / n),
          r=[ss[1], epsc], w=[rs[1]])
    kb.op("dve", lambda g: g.reciprocal(out=rs[0], in_=rs[0]), r=[rs[1]], w=[rs[1]])


def phase_a(kb, D):
    with ExitStack() as es:
        sb = lambda n, s, d: kb.sb(es, n, s, d)
        ps = lambda n, s, d: kb.ps(es, n, s, d)
        w_in = sb("w_in", [128, 8, 5792], BF16)
        w_uq = sb("w_uq", [128, 3, 768], BF16)
        w_ukv = sb("w_ukv", [128, 2, 1024], BF16)
        ident = sb("identA", [128, 128], BF16)
        gmix = sb("gmix", [128, 1024], F32)
        gql = sb("gql", [128, 384], F32)
        gkvl = sb("gkvl", [128, 256], F32)
        gq = sb("gq", [128, 96], F32)
        gk = sb("gk", [128, 96], F32)
        cosm = sb("cosm", [128, NT, 16], F32)
        sinm = sb("sinm", [128, NT, 16], F32)
        cosr = sb("cosr", [128, NT, 32], F32)
        sinr = sb("sinr", [128, NT, 32], F32)
        epsc = sb("epscA", [128, 1], F32)
        xt = [sb("xt%d" % i, [128, 1024], F32) for i in range(2)]
        junk = sb("junkA", [128, 1024], BF16)
        ss = sb("ssA", [128, 1], F32)
        rs = sb("rsA", [128, 1], F32)
        hn = sb("hn", [128, 1024], BF16)
        hnT = sb("hnT", [128, 1024], BF16)
        cqn = sb("cqn", [128, 384], BF16)
        cqT = sb("cqT", [128, 384], BF16)
        ckvn = sb("ckvn", [128, 256], BF16)
        ckvT = sb("ckvT", [128, 256], BF16)
        sq = sb("sqA", [128, 1024], F32)
        st8 = sb("st8", [128, 8], F32)
        rs8 = sb("rs8", [128, 8], F32)
        ss1 = sb("ss1", [128, 1], F32)
        qn = sb("qn", [128, 8, 96], F32)
        qf = sb("qf", [128, 8, 96], BF16)
        kf = sb("kf", [128, 8, 96], BF16)
        kpg = sb("kpg", [128, 32], F32)
        rk = sb("rkA", [128, 32], F32)
        ta = sb("taA", [128, 8, 32], F32)
        tb = sb("tbA", [128, 8, 32], F32)
        tc = sb("tcA", [128, 8, 32], F32)
        td = sb("tdA", [128, 8, 32], F32)
        qT = sb("qTsb", [96, 8, 128], BF16)
        kT = sb("kTsb", [96, 8, 128], BF16)
        va = sb("vaA", [128, 8, 65], BF16)
        rqf = sb("rqf", [128, 8, 64], BF16)
        rkf = sb("rkf", [128, 8, 64], BF16)
        rqT = sb("rqTsb", [128, 4, 128], BF16)
        rkT = sb("rkTsb", [128, 4, 128], BF16)
        ob = [sb("obA%d" % i, [128, 512], BF16) for i in range(4)]
        zp = [ps("zp%d" % i, [128, 512], F32) for i in range(3)]
        tpa = ps("tpa", [128, 1024], BF16)
        tpb = ps("tpb", [128, 1024], BF16)
        qp = ps("qp", [128, 1024], F32)

        for k in range(8):
            for c0, c1 in ((0, 2048), (2048, 4096), (4096, 5792)):
                kb.dma("pool", w_in[:, k, c0:c1], D["w_in"][k * 128:(k + 1) * 128, c0:c1], pw=[w_in])
        for k in range(3):
            kb.dma("pool", w_uq[:, k, :], D["w_uq"][k * 128:(k + 1) * 128, :], pw=[w_uq])
        for k in range(2):
            kb.dma("pool", w_ukv[:, k, :], D["w_ukv"][k * 128:(k + 1) * 128, :], pw=[w_ukv])
        kb.dma("pool", ident[:], D["ident"][:, :], w=[ident])
        for t_, n_ in ((gmix, "g_mix"), (gql, "g_q_lora"), (gkvl, "g_kv_lora"), (gq, "g_qk_q"), (gk, "g_qk_k"),
                       (epsc, "epsc")):
            kb.dma("sp", t_[:], D[n_][:, :], w=[t_])
        for t_, n_ in ((cosm, "cosm"), (sinm, "sinm"), (cosr, "cosr"), (sinr, "sinr")):
            kb.dma("sp", t_[:].rearrange("p t d -> p (t d)"), D[n_][:, :], w=[t_])
        kb.op("pool", lambda g: g.memset(va[:].rearrange("p h d -> p (h d)"), 1.0), w=[va])

        sq1 = sb("sq1A", [128, 1024], F32)
        ss1b = sb("ss1bA", [128, 1], F32)
        ss2 = sb("ss2A", [128, 1], F32)
        st8b = sb("st8bA", [128, 8], F32)
        rs8b = sb("rs8bA", [128, 8], F32)
        qn1 = sb("qn1A", [128, 8, 96], F32)
        ta1 = sb("ta1A", [128, 8, 32], F32)
        tb1 = sb("tb1A", [128, 8, 32], F32)

        def post0(z, T):
            kb.op("act", lambda g: g.activation(out=sq[:, 0:384], in_=z[:, 0:384], func=AF.Square,
                                                accum_out=ss1[:, 0:1]), r=[z], w=[sq, ss1])
            rstd_from_ss(kb, (ss1[:, 0:1], ss1), (ss1[:, 0:1], ss1), epsc, 384.0)
            kb.op("dve", lambda g: g.scalar_tensor_tensor(out=cqn[:], in0=z[:, 0:384], scalar=ss1[:, 0:1],
                                                          in1=gql[:], op0=ALU.mult, op1=ALU.mult),
                  r=[z, ss1, gql], w=[cqn])
            yield
            for k in range(3):
                kb.op("pe", lambda g: g.transpose(out=tpb[:, k * 128:(k + 1) * 128],
                                                  in_=cqn[:, k * 128:(k + 1) * 128], identity=ident[:]),
                      r=[cqn, ident], w=[tpb])
            kb.op("dve", lambda g: g.tensor_copy(out=cqT[:], in_=tpb[:, 0:384]), r=[tpb], w=[cqT])
            yield
            for (a0, a1) in ((0, 512), (512, 768)):
                for k in range(3):
                    kb.op("pe", lambda g: g.matmul(qp[:, a0:a1], lhsT=cqT[:, k * 128:(k + 1) * 128],
                                                   rhs=w_uq[:, k, a0:a1], start=(k == 0), stop=(k == 2)),
                          r=[cqT, w_uq], w=[qp])
            kb.op("act", lambda g: g.activation(out=sq[:, 0:768], in_=qp[:, 0:768], func=AF.Square),
                  r=[qp], w=[sq])
            kb.op("dve", lambda g: g.tensor_reduce(out=st8[:], in_=sq[:, 0:768].rearrange("p (h d) -> p h d", h=8),
                                                   axis=AX.X, op=ALU.add), r=[sq], w=[st8])
            rstd_from_ss(kb, (st8[:], st8), (rs8[:], rs8), epsc, 96.0)
            kb.op("dve", lambda g: g.tensor_tensor(out=qn[:], in0=qp[:, 0:768].rearrange("p (h d) -> p h d", h=8),
                                                   in1=rs8[:].unsqueeze(2).to_broadcast([128, 8, 96]),
                                                   op=ALU.mult), r=[qp, rs8], w=[qn])
            yield
            kb.op("pool", lambda g: g.tensor_tensor(out=qn[:], in0=qn[:],
                                                    in1=gq[:].unsqueeze(1).to_broadcast([128, 8, 96]),
                                                    op=ALU.mult), r=[qn, gq], w=[qn])
            kb.op("pool", lambda g: g.tensor_copy(out=qf[:, :, 0:64], in_=qn[:, :, 0:64]), r=[qn], pw=[qf])
            cs = cosm[:, T, :].unsqueeze(1).to_broadcast([128, 8, 16])
            sn = sinm[:, T, :].unsqueeze(1).to_broadcast([128, 8, 16])
            rope(kb, "pool", qf[:, :, 64:80], qf[:, :, 80:96], qn[:, :, 64:80], qn[:, :, 80:96], cs, sn,
                 (ta, ta[:, :, 0:16]), (tb, tb[:, :, 0:16]), [qn, cosm, sinm], [qf])
            yield
            yield
            for h in range(8):
                kb.op("pe", lambda g: g.transpose(out=tpb[0:96, h * 128:(h + 1) * 128], in_=qf[:, h, :],
                                                  identity=ident[:]), r=[qf, ident], w=[tpb])
            kb.op("act", lambda g: g.copy(out=qT[:].rearrange("p h t -> p (h t)"), in_=tpb[0:96, :]),
                  r=[tpb], w=[qT])
            kb.dma("sp", D["QT"][:, :, T * 128:(T + 1) * 128].rearrange("h d t -> d h t"), qT[:],
                   r=[qT], pw=[D["QT"]])
            yield

        def post1(z, T):
            kb.op("act", lambda g: g.activation(out=sq1[:, 0:256], in_=z[:, 0:256], func=AF.Square,
                                                accum_out=ss1b[:, 0:1]), r=[z], w=[sq1, ss1b])
            rstd_from_ss(kb, (ss1b[:, 0:1], ss1b), (ss1b[:, 0:1], ss1b), epsc, 256.0)
            kb.op("dve", lambda g: g.scalar_tensor_tensor(out=ckvn[:], in0=z[:, 0:256], scalar=ss1b[:, 0:1],
                                                          in1=gkvl[:], op0=ALU.mult, op1=ALU.mult),
                  r=[z, ss1b, gkvl], w=[ckvn])
            kb.op("act", lambda g: g.activation(out=sq1[:, 0:32], in_=z[:, 256:288], func=AF.Square,
                                                accum_out=ss2[:, 0:1]), r=[z], w=[sq1, ss2])
            kb.op("dve", lambda g: g.tensor_tensor(out=kpg[:], in0=z[:, 256:288], in1=gk[:, 64:96], op=ALU.mult),
                  r=[z, gk], w=[kpg])
            yield
            for k in range(2):
                kb.op("pe", lambda g: g.transpose(out=tpb[:, k * 128:(k + 1) * 128],
                                                  in_=ckvn[:, k * 128:(k + 1) * 128], identity=ident[:]),
                      r=[ckvn, ident], w=[tpb])
            kb.op("dve", lambda g: g.tensor_copy(out=ckvT[:], in_=tpb[:, 0:256]), r=[tpb], w=[ckvT])
            yield
            for (a0, a1) in ((0, 512), (512, 1024)):
                for k in range(2):
                    kb.op("pe", lambda g: g.matmul(qp[:, a0:a1], lhsT=ckvT[:, k * 128:(k + 1) * 128],
                                                   rhs=w_ukv[:, k, a0:a1], start=(k == 0), stop=(k == 1)),
                          r=[ckvT, w_ukv], w=[qp])
            kv3 = qp[:, 0:1024].rearrange("p (h d) -> p h d", h=8)
            kb.op("act", lambda g: g.activation(out=sq1[:, 0:512].rearrange("p (h d) -> p h d", h=8),
                                                in_=kv3[:, :, 0:64], func=AF.Square), r=[qp], w=[sq1])
            kb.op("dve", lambda g: g.tensor_reduce(out=st8b[:], in_=sq1[:, 0:512].rearrange("p (h d) -> p h d", h=8),
                                                   axis=AX.X, op=ALU.add), r=[sq1], w=[st8b])
            kb.op("dve", lambda g: g.tensor_scalar(out=st8b[:], in0=st8b[:], scalar1=ss2[:, 0:1], scalar2=None,
                                                   op0=ALU.add), r=[st8b, ss2], w=[st8b])
            rstd_from_ss(kb, (st8b[:], st8b), (rs8b[:], rs8b), epsc, 96.0)
            kb.op("dve", lambda g: g.tensor_tensor(out=qn1[:, :, 0:64], in0=kv3[:, :, 0:64],
                                                   in1=rs8b[:].unsqueeze(2).to_broadcast([128, 8, 64]),
                                                   op=ALU.mult), r=[qp, rs8b], w=[qn1])
            kb.op("act", lambda g: g.copy(out=va[:, :, 0:64], in_=kv3[:, :, 64:128]), r=[qp], w=[va])
            kb.dma("sp", D["VA"][T * 128:(T + 1) * 128, :], va[:].rearrange("p h d -> p (h d)"),
                   r=[va], pw=[D["VA"]])
            yield
            kb.op("pool", lambda g: g.tensor_tensor(out=kf[:, :, 0:64], in0=qn1[:, :, 0:64],
                                                    in1=gk[:, 0:64].unsqueeze(1).to_broadcast([128, 8, 64]),
                                                    op=ALU.mult), r=[qn1, gk], pw=[kf])
            rope(kb, "pool", rk[:, 0:16], rk[:, 16:32], kpg[:, 0:16], kpg[:, 16:32], cosm[:, T, :], sinm[:, T, :],
                 (ta1, ta1[:, 0, 0:16]), (tb1, tb1[:, 0, 0:16]), [kpg, cosm, sinm], [rk])
            kb.op("pool", lambda g: g.tensor_tensor(out=kf[:, :, 64:96],
                                                    in0=rk[:].unsqueeze(1).to_broadcast([128, 8, 32]),
                                                    in1=rs8b[:].unsqueeze(2).to_broadcast([128, 8, 32]),
                                                    op=ALU.mult), r=[rk, rs8b], pw=[kf])
            yield
            yield
            for h in range(8):
                kb.op("pe", lambda g: g.transpose(out=tpb[0:96, h * 128:(h + 1) * 128], in_=kf[:, h, :],
                                                  identity=ident[:]), r=[kf, ident], w=[tpb])
            kb.op("act", lambda g: g.copy(out=kT[:].rearrange("p h t -> p (h t)"), in_=tpb[0:96, :]),
                  r=[tpb], w=[kT])
            kb.dma("sp", D["KT"][:, :, T * 128:(T + 1) * 128].rearrange("h d t -> d h t"), kT[:],
                   r=[kT], pw=[D["KT"]])
            yield

        def post23(gi, z, T):
            dst = rqf if gi == 2 else rkf
            z3 = z[:, 0:512].rearrange("p (h d) -> p h d", h=8)
            cs = cosr[:, T, :].unsqueeze(1).to_broadcast([128, 8, 32])
            sn = sinr[:, T, :].unsqueeze(1).to_broadcast([128, 8, 32])
            rope(kb, "dve", dst[:, :, 0:32], dst[:, :, 32:64], z3[:, :, 0:32], z3[:, :, 32:64], cs, sn,
                 (tc, tc[:]), (td, td[:]), [z, cosr, sinr], [dst])
            yield
            yield
            dT = rqT if gi == 2 else rkT
            for j in range(4):
                kb.op("pe", lambda g: g.transpose(out=tpb[:, j * 128:(j + 1) * 128],
                                                  in_=dst[:, 2 * j:2 * j + 2, :].rearrange("p h d -> p (h d)"),
                                                  identity=ident[:]), r=[dst, ident], w=[tpb])
            kb.op("act", lambda g: g.copy(out=dT[:].rearrange("p j t -> p (j t)"), in_=tpb[:, 0:512]),
                  r=[tpb], w=[dT])
            name = "RQT" if gi == 2 else "RKT"
            kb.dma("sp", D[name][:, :, T * 128:(T + 1) * 128].rearrange("(j s) d t -> (s d) j t", s=2), dT[:],
                   r=[dT], pw=[D[name]])
            if gi == 3:
                kb.dma("sp", D["RK"][T * 128:(T + 1) * 128, :], dst[:].rearrange("p h d -> p (h d)"),
                       r=[dst], pw=[D["RK"]])
            yield

        pending = []

        for T in range(NT):
            x_ = xt[T % 2]
            kb.dma("sp", x_[:], D["xpad"][T * 128:(T + 1) * 128, :], w=[x_])
            kb.op("act", lambda g: g.activation(out=junk[:], in_=x_[:], func=AF.Square, accum_out=ss[:, 0:1]),
                  r=[x_], w=[junk, ss])
            rstd_from_ss(kb, (ss[:, 0:1], ss), (rs[:, 0:1], rs), epsc, 1024.0)
            kb.op("dve", lambda g: g.scalar_tensor_tensor(out=hn[:], in0=x_[:], scalar=rs[:, 0:1], in1=gmix[:],
                                                          op0=ALU.mult, op1=ALU.mult),
                  r=[x_, rs, gmix], w=[hn])
            for k in range(8):
                kb.op("pe", lambda g: g.transpose(out=tpa[:, k * 128:(k + 1) * 128], in_=hn[:, k * 128:(k + 1) * 128],
                                                  identity=ident[:]), r=[hn, ident], w=[tpa])
            kb.op("act", lambda g: g.copy(out=hnT[:], in_=tpa[:]), r=[tpa], w=[hnT])

            for gi, (c0, c1) in enumerate(GROUPS):
                n = c1 - c0
                z = zp[gi % 3]
                for k in range(8):
                    kb.op("pe", lambda g: g.matmul(z[:, 0:n], lhsT=hnT[:, k * 128:(k + 1) * 128], rhs=w_in[:, k, c0:c1],
                                                   start=(k == 0), stop=(k == 7)), r=[hnT, w_in], w=[z])
                if gi == 0:
                    pending.append(post0(z, T))
                elif gi == 1:
                    pending.append(post1(z, T))
                elif gi in (2, 3):
                    pending.append(post23(gi, z, T))
                else:
                    o = ob[gi % 4]
                    if gi in (4, 5):
                        kb.op("dve", lambda g: g.tensor_copy(out=o[:], in_=z[:, 0:512]), r=[z], w=[o])
                        name, cc = "RV", (gi - 4) * 512
                    elif gi in (6, 7):
                        kb.op("act", lambda g: g.activation(out=o[:], in_=z[:, 0:512], func=AF.Silu), r=[z], w=[o])
                        name, cc = "RG", (gi - 6) * 512
                    elif gi in (8, 9):
                        kb.op("act", lambda g: g.activation(out=o[:], in_=z[:, 0:512], func=AF.Sigmoid), r=[z], w=[o])
                        name, cc = "ZA", (gi - 8) * 512
                    else:
                        kb.op("act", lambda g: g.activation(out=o[:], in_=z[:, 0:512], func=AF.Sigmoid), r=[z], w=[o])
                        name, cc = "ZR", (gi - 10) * 512
                    kb.dma("sp", D[name][T * 128:(T + 1) * 128, cc:cc + 512], o[:], r=[o], pw=[D[name]])
                for gen in list(pending):
                    if next(gen, "done") == "done":
                        pending.remove(gen)
        for gen in pending:
            for _ in gen:
                pass
        kb.barrier()


def phase_b(kb, D):
    scale = 1.0 / math.sqrt(96.0)
    with ExitStack() as es:
        sb = lambda n, s, d: kb.sb(es, n, s, d)
        ps = lambda n, s, d: kb.ps(es, n, s, d)
        tri = sb("tri", [128, 128], BF16)
        kT = [sb("kTB%d" % i, [96, LP], BF16) for i in range(2)]
        qT = [sb("qTB%d" % i, [96, LP], BF16) for i in range(2)]
        vh = [sb("vhB%d" % i, [128, NT, 65], BF16) for i in range(2)]
        vm = [sb("vmB%d" % i, [16, 65], BF16) for i in range(2)]
        pT = [sb("pT%d" % i, [128, 512], BF16) for i in range(3)]
        ya = sb("yaB", [128, 32, 512], BF16)
        rc = sb("rcB", [128, 1], F32)
        sp_ = [ps("spB%d" % i, [128, 512], F32) for i in range(3)]
        op_ = [ps("opB%d" % i, [128, 512], F32) for i in range(4)]
        kb.dma("pool", tri[:], D["tri"][:, :], w=[tri])
        VAr = D["VA"].t.rearrange("(t p) c -> p t c", p=128)
        cvb = [sb("cvb%d" % i, [128, 4096], BF16) for i in range(2)]
        conv = [(src, off, c) for (src, off) in (("peer_u", 0), ("peer_v", 1024)) for c in range(32)]

        def convert(n):
            for _ in range(n):
                if not conv:
                    return
                src, off, c = conv.pop(0)
                cb = cvb[c % 2]
                sv = D[src].t.rearrange("(c p r) d -> c p (r d)", p=128, r=4)
                dv = D["UVB"].t.rearrange("(c p r) d -> c p r d", p=128, r=4)
                for a0 in (0, 2048):
                    kb.dma("pool", cb[:, a0:a0 + 2048], sv[c, :, a0:a0 + 2048], pw=[cb])
                kb.dma("sp", dv[c, :, :, off:off + 1024], cb[:].rearrange("p (r d) -> p r d", r=4), r=[cb], pw=[D["UVB"]])
        def head_loads(h):
            b = h % 2
            kb.dma("sp", kT[b][:], D["KT"][h, :, :], r=[D["KT"]], w=[kT[b]])
            kb.dma("sp", qT[b][:], D["QT"][h, :, :], r=[D["QT"]], w=[qT[b]])
            for t0 in range(0, NT, 11):
                kb.dma("sp", vh[b][:, t0:t0 + 11, :], VAr[:, t0:t0 + 11, h * 65:(h + 1) * 65], r=[D["VA"]], pw=[vh[b]])
            kb.dma("sp", vm[b][:], D["VA"][112:128, h * 65:(h + 1) * 65], r=[D["VA"]], w=[vm[b]])

        its = [(h, Q, kt) for h in range(8) for Q in range(8) for kt in range(0, 4 * Q + 5)]

        def geom(n):
            h, Q, kt = its[n]
            j0 = max(0, kt - (4 * Q + 1))
            return h, Q, kt, h % 2, (4 * Q + 1) * 128, j0, 128 * j0, (16 if kt == 0 else 128), sp_[n % 3], pT[n % 3]

        def emit_s(n):
            h, Q, kt, b, qc0, j0, c0, nk, s_, p_ = geom(n)
            if Q == 0 and kt == 0:
                head_loads(h)
            lhs = kT[b][:, 112:128] if kt == 0 else kT[b][:, kt * 128:(kt + 1) * 128]
            kb.op("pe", lambda g: g.matmul(s_[0:nk, c0:512], lhsT=lhs, rhs=qT[b][:, qc0 + c0:qc0 + 512],
                                           start=True, stop=True), r=[kT[b], qT[b]], w=[s_])

        def emit_rest(n):
            h, Q, kt, b, qc0, j0, c0, nk, s_, p_ = geom(n)
            if kt == 0:
                convert(1)
            kb.op("act", lambda g: g.activation(out=p_[0:nk, c0:512], in_=s_[0:nk, c0:512], func=AF.Exp,
                                                scale=scale), r=[s_], w=[p_])
            if kt >= 4 * Q + 1:
                kb.op("dve", lambda g: g.tensor_tensor(out=p_[:, c0:c0 + 128], in0=p_[:, c0:c0 + 128],
                                                       in1=tri[:], op=ALU.mult), r=[p_, tri], w=[p_])
            rhs = vm[b][:, :] if kt == 0 else vh[b][:, kt, :]
            for j in range(j0, 4):
                kb.op("pe", lambda g: g.matmul(op_[j][:, 0:65], lhsT=p_[0:nk, j * 128:(j + 1) * 128], rhs=rhs,
                                               start=(kt == 0), stop=(kt == 4 * Q + 1 + j)),
                      r=[p_, vh[b], vm[b]], w=[op_[j]])
            if kt == 4 * Q + 4:
                for j in range(4):
                    kb.op("dve", lambda g: g.reciprocal(out=rc[:, 0:1], in_=op_[j][:, 64:65]), r=[op_[j]], w=[rc])
                    kb.op("dve", lambda g: g.tensor_scalar(out=ya[:, 4 * Q + j, h * 64:(h + 1) * 64], in0=op_[j][:, 0:64],
                                                           scalar1=rc[:, 0:1], scalar2=None, op0=ALU.mult),
                          r=[op_[j], rc], pw=[ya])

        emit_s(0)
        for n in range(len(its)):
            if n + 1 < len(its):
                emit_s(n + 1)
            emit_rest(n)
        convert(64)
        kb.dma("sp", D["YA"].t.rearrange("(t p) c -> p t c", p=128), ya[:], r=[ya], w=[D["YA"]])
        kb.barrier()


def phase_c(kb, D, gam):
    with ExitStack() as es:
        sb = lambda n, s, d: kb.sb(es, n, s, d)
        ps = lambda n, s, d: kb.ps(es, n, s, d)
        NB = 2
        rqT = [sb("rqTC%d" % i, [64, LP], BF16) for i in range(NB)]
        rkT = [sb("rkTC%d" % i, [64, LP], BF16) for i in range(NB)]
        rk = [sb("rkC%d" % i, [128, NT, 64], BF16) for i in range(NB)]
        rv = [sb("rvC%d" % i, [128, NT, 128], BF16) for i in range(NB)]
        rg = [sb("rgC%d" % i, [128, NT, 128], BF16) for i in range(NB)]
        yr = [sb("yrC%d" % i, [128, NT, 128], BF16) for i in range(NB)]
        dec = [sb("decC%d" % i, [128, 128], F32) for i in range(NB)]
        xi = [sb("xiC%d" % i, [64, 128], F32) for i in range(NB)]
        zeta = sb("zetaC", [128, 8], F32)
        ggn = sb("ggnC", [128, 1024], F32)
        epsc = sb("epscC", [128, 1], F32)
        S = [sb("SC%d" % i, [64, 128], F32) for i in range(NB)]
        Sb = [sb("SbC%d" % i, [64, 128], BF16) for i in range(NB)]
        sTd = [sb("sTd%d" % i, [128, 128], BF16) for i in range(NB)]
        qxi = [sb("qxi%d" % i, [64, 128], BF16) for i in range(NB)]
        kz = [sb("kz%d" % i, [128, 64], BF16) for i in range(NB)]
        bst = [sb("bst%d" % i, [128, 6], F32) for i in range(NB)]
        mv = [sb("mv%d" % i, [128, 2], F32) for i in range(NB)]
        rsd = [sb("rsd%d" % i, [128, 1], F32) for i in range(NB)]
        nmr = [sb("nmr%d" % i, [128, 1], F32) for i in range(NB)]
        yn = [sb("ynC%d" % i, [128, 128], F32) for i in range(NB)]
        sps = [ps("spsC%d" % i, [128, 512], F32) for i in range(NB)]
        yps = [ps("ypsC%d" % i, [128, 512], F32) for i in range(NB)]
        kvps = [ps("kvpsC%d" % i, [128, 512], F32) for i in range(NB)]
        kb.dma("sp", zeta[:], D["zeta"][:, :], w=[zeta])
        kb.dma("sp", ggn[:], D["g_ret_gn"][:, :], w=[ggn])
        kb.dma("sp", epsc[:], D["epsc"][:, :], w=[epsc])
        steps = [(hp, n, s) for hp in range(4) for n in range(NT) for s in range(2)]

        def slot_loads(hp, s):
            h = 2 * hp + s
            kb.dma("sp", rqT[s][:], D["RQT"][h, :, :], r=[D["RQT"]], w=[rqT[s]])
            kb.dma("sp", rkT[s][:], D["RKT"][h, :, :], r=[D["RKT"]], w=[rkT[s]])
            for t0 in range(0, NT, 11):
                kb.dma("sp", rk[s][:, t0:t0 + 11, :],
                       D["RK"].t.rearrange("(t p) c -> p t c", p=128)[:, t0:t0 + 11, h * 64:(h + 1) * 64],
                       r=[D["RK"]], pw=[rk[s]])
                kb.dma("sp", rv[s][:, t0:t0 + 11, :],
                       D["RV"].t.rearrange("(t p) c -> p t c", p=128)[:, t0:t0 + 11, h * 128:(h + 1) * 128],
                       r=[D["RV"]], pw=[rv[s]])
                kb.dma("sp", rg[s][:, t0:t0 + 11, :],
                       D["RG"].t.rearrange("(t p) c -> p t c", p=128)[:, t0:t0 + 11, h * 128:(h + 1) * 128],
                       r=[D["RG"]], pw=[rg[s]])
            kb.dma("sp", dec[s][:], D["decT"][h, :, :], w=[dec[s]])
            kb.dma("sp", xi[s][:], D["xi"][h, :, :], w=[xi[s]])

        def emit_pre(i):
            hp, n, s = steps[i]
            h = 2 * hp + s
            cs = slice(n * 128, (n + 1) * 128)
            if n == 0:
                slot_loads(hp, s)
            kb.op("pe", lambda g: g.matmul(sps[s][:, 0:128], lhsT=rkT[s][:, cs], rhs=rqT[s][:, cs],
                                           start=True, stop=True), r=[rkT[s], rqT[s]], w=[sps[s]])
            if n > 0:
                kb.op("pool", lambda g: g.tensor_tensor(out=qxi[s][:], in0=rqT[s][:, cs], in1=xi[s][:],
                                                        op=ALU.mult), r=[rqT[s], xi[s]], w=[qxi[s]])
            if n < NT - 1:
                kb.op("pool", lambda g: g.tensor_scalar(out=kz[s][:], in0=rk[s][:, n, :], scalar1=zeta[:, h:h + 1],
                                                        scalar2=None, op0=ALU.mult), r=[rk[s], zeta], w=[kz[s]])

        def emit_rest(i):
            hp, n, s = steps[i]
            h = 2 * hp + s
            cs = slice(n * 128, (n + 1) * 128)
            if n == NT - 1:
                kb_out = True
            kb.op("dve", lambda g: g.tensor_tensor(out=sTd[s][:], in0=sps[s][:, 0:128], in1=dec[s][:],
                                                   op=ALU.mult), r=[sps[s], dec[s]], w=[sTd[s]])
            kb.op("pe", lambda g: g.matmul(yps[s][:, 0:128], lhsT=sTd[s][:], rhs=rv[s][:, n, :],
                                           start=True, stop=(n == 0)), r=[sTd[s], rv[s]], w=[yps[s]])
            if n > 0:
                kb.op("pe", lambda g: g.matmul(yps[s][:, 0:128], lhsT=qxi[s][:], rhs=Sb[s][:],
                                               start=False, stop=True), r=[qxi[s], Sb[s]], w=[yps[s]])
            if n < NT - 1:
                kb.op("pe", lambda g: g.matmul(kvps[s][0:64, 0:128], lhsT=kz[s][:], rhs=rv[s][:, n, :],
                                               start=True, stop=True), r=[kz[s], rv[s]], w=[kvps[s]])
                if n == 0:
                    kb.op("dve", lambda g: g.tensor_copy(out=S[s][:], in_=kvps[s][0:64, 0:128]),
                          r=[kvps[s]], w=[S[s]])
                else:
                    kb.op("dve", lambda g: g.scalar_tensor_tensor(out=S[s][:], in0=S[s][:], scalar=float(gam[h]),
                                                                  in1=kvps[s][0:64, 0:128], op0=ALU.mult,
                                                                  op1=ALU.add), r=[S[s], kvps[s]], w=[S[s]])
                kb.op("act", lambda g: g.copy(out=Sb[s][:], in_=S[s][:]), r=[S[s]], w=[Sb[s]])
            if n == 0:
                return
            kb.op("dve", lambda g: g.bn_stats(out=bst[s][:], in_=yps[s][:, 0:128]), r=[yps[s]], w=[bst[s]])
            kb.op("dve", lambda g: g.bn_aggr(out=mv[s][:], in_=bst[s][:]), r=[bst[s]], w=[mv[s]])
            kb.op("act", lambda g: g.activation(out=rsd[s][:], in_=mv[s][:, 1:2], func=AF.Sqrt,
                                                bias=epsc[:, 0:1], scale=1.0), r=[mv[s], epsc], w=[rsd[s]])
            kb.op("dve", lambda g: g.reciprocal(out=rsd[s][:], in_=rsd[s][:]), r=[rsd[s]], w=[rsd[s]])
            kb.op("dve", lambda g: g.scalar_tensor_tensor(out=nmr[s][:], in0=mv[s][:, 0:1], scalar=-1.0,
                                                          in1=rsd[s][:], op0=ALU.mult, op1=ALU.mult),
                  r=[mv[s], rsd[s]], w=[nmr[s]])
            kb.op("act", lambda g: g.activation(out=yn[s][:], in_=yps[s][:, 0:128], func=AF.Identity,
                                                bias=nmr[s][:, 0:1], scale=rsd[s][:, 0:1]),
                  r=[yps[s], nmr[s], rsd[s]], w=[yn[s]])
            kb.op("pool", lambda g: g.tensor_tensor(out=yn[s][:], in0=yn[s][:], in1=ggn[:, h * 128:(h + 1) * 128],
                                                    op=ALU.mult), r=[yn[s], ggn], w=[yn[s]])
            kb.op("pool", lambda g: g.tensor_tensor(out=yr[s][:, n, :], in0=yn[s][:], in1=rg[s][:, n, :],
                                                    op=ALU.mult), r=[yn[s], rg[s]], pw=[yr[s]])

        def emit_out(i):
            hp, n, s = steps[i]
            h = 2 * hp + s
            if n == NT - 1:
                kb.dma("sp", D["YR"].t.rearrange("(t p) c -> p t c", p=128)[:, 1:NT, h * 128:(h + 1) * 128],
                       yr[s][:, 1:NT, :], r=[yr[s]], pw=[D["YR"]])

        emit_pre(0)
        for i in range(len(steps)):
            if i + 1 < len(steps):
                emit_pre(i + 1)
            emit_rest(i)
            emit_out(i)
        kb.barrier()


def phase_d(kb, D, tiles, stop=0):
    with ExitStack() as es:
        sb = lambda n, s, d: kb.sb(es, n, s, d)
        ps = lambda n, s, d: kb.ps(es, n, s, d)
        w_mo = sb("w_mo", [128, 4, 1024], BF16)
        w_ro = sb("w_ro", [128, 8, 1024], BF16)
        w_xo = sb("w_xo", [128, 8, 1024], BF16)
        w_pq = sb("w_pq", [128, 8, 1024], BF16)
        keys = sb("keysD", [128, 256], BF16)
        ident = sb("identD", [128, 128], BF16)
        zc = sb("zcD", [128, 255], F32)
        gffn = sb("gffn", [128, 1024], F32)
        epsc = sb("epscD", [128, 1], F32)
        xt = sb("xtD", [128, 1024], F32)
        ya = sb("yaD", [128, 512], BF16)
        yr = sb("yrD", [128, 1024], BF16)
        za = sb("zaD", [128, 1024], BF16)
        zr = sb("zrD", [128, 1024], BF16)
        yaT = sb("yaTD", [128, 512], BF16)
        yrT = sb("yrTD", [128, 1024], BF16)
        m1 = sb("m1D", [128, 1024], F32)
        m2 = sb("m2D", [128, 1024], F32)
        mg = sb("mgD", [128, 1024], BF16)
        mgT = sb("mgTD", [128, 1024], BF16)
        hh2 = [sb("hhD%d" % i, [128, 1024], F32) for i in range(2)]
        xn2 = [sb("xnD%d" % i, [128, 1024], BF16) for i in range(2)]
        eT2 = [sb("eTD%d" % i, [128, 128], I32) for i in range(2)]
        gT2 = [sb("gTD%d" % i, [128, 128], F32) for i in range(2)]
        junk = sb("junkD", [128, 1024], BF16)
        junkr = sb("junkrD", [128, 1024], BF16)
        ss = sb("ssD", [128, 1], F32)
        rs = sb("rsD", [128, 1], F32)
        xnT = sb("xnTD", [128, 1024], BF16)
        pqT = sb("pqTD", [128, 1024], BF16)
        sc = sb("scD", [128, 8, 256], F32)
        scm = sb("scmD", [128, 8, 256], F32)
        v12 = sb("v12D", [128, 8, 2, 16], F32)
        i12 = sb("i12D", [128, 8, 2, 16], U32)
        i12f = sb("i12fD", [128, 8, 2, 16], F32)
        cand = sb("candD", [128, 8, 256], F32)
        candm = sb("candmD", [128, 8, 256], F32)
        cv = sb("cvD", [128, 8, 16], F32)
        ci = sb("ciD", [128, 8, 16], U32)
        oh = sb("ohD", [128, 8, 16, 16], F32)
        af = sb("afD", [128, 8, 16], F32)
        bf = sb("bfD", [128, 8, 16], F32)
        iota = sb("iotaD", [128, 32], F32)
        e1b = sb("e1bD", [128, 128], BF16)
        e2b = sb("e2bD", [128, 128], BF16)
        ghi = sb("ghiD", [128, 128], BF16)
        glo = sb("gloD", [128, 128], BF16)
        tsb = sb("tsbD", [128, 512], BF16)
        cf = sb("cfD", [128, 8, 16], F32)
        ef = sb("efD", [128, 128], F32)
        es_ = sb("esD", [128, 8, 16], F32)
        sm8 = sb("sm8D", [128, 8], F32)
        gts = sb("gtsD", [128, 128], F32)
        NG = 8
        uvs = [sb("uvs%d" % i, [128, 2048], BF16) for i in range(NG)]
        acol = [sb("acol%d" % i, [128, 1], F32) for i in range(NG)]
        wcol = [sb("wcol%d" % i, [128, 1], F32) for i in range(NG)]
        selb = [sb("selD%d" % i, [128, 128], BF16) for i in range(NG)]
        G = [sb("GD%d" % i, [128, 128], BF16) for i in range(NG)]
        ot = sb("otD", [128, 1024], F32)
        pA = ps("pA", [128, 1024], F32)
        pB = ps("pB", [128, 1024], F32)
        pC = ps("pC", [128, 1024], F32)
        pT = ps("pTD", [128, 1024], BF16)
        pF = ps("pFD", [128, 512], F32)

        for k in range(4):
            kb.dma("pool", w_mo[:, k, :], D["w_mla_out"][k * 128:(k + 1) * 128, :], pw=[w_mo])
        for t_, n_ in ((w_ro, "w_ret_out"), (w_xo, "w_mix_out"), (w_pq, "w_peer_q")):
            for k in range(8):
                kb.dma("pool", t_[:, k, :], D[n_][k * 128:(k + 1) * 128, :], pw=[t_])
        kb.dma("pool", keys[:], D["keysbd"][:, :], w=[keys])
        kb.dma("pool", ident[:], D["ident"][:, :], w=[ident])
        kb.dma("sp", zc[:], D["zc"][:, :], w=[zc])
        kb.dma("sp", iota[:], D["iota16"][:, :], w=[iota])
        kb.dma("sp", gffn[:], D["g_ffn"][:, :], w=[gffn])
        kb.dma("sp", epsc[:], D["epsc"][:, :], w=[epsc])

        def route(T):
            r0 = T * 128
            hh, xn, eT, gT = hh2[T % 2], xn2[T % 2], eT2[T % 2], gT2[T % 2]
            kb.dma("sp", xt[:], D["xpad"][r0:r0 + 128, :], w=[xt])
            kb.dma("sp", ya[:], D["YA"][r0 - 128:r0, :], r=[D["YA"]], w=[ya])
            kb.dma("sp", yr[:], D["YR"][r0:r0 + 128, :], r=[D["YR"]], w=[yr])
            kb.dma("sp", za[:], D["ZA"][r0:r0 + 128, :], r=[D["ZA"]], w=[za])
            kb.dma("sp", zr[:], D["ZR"][r0:r0 + 128, :], r=[D["ZR"]], w=[zr])
            yield

            def transposes(src, dst, nk):
                for k in range(nk):
                    kb.op("pe", lambda g: g.transpose(out=pT[:, k * 128:(k + 1) * 128], in_=src[:, k * 128:(k + 1) * 128],
                                                      identity=ident[:]), r=[src, ident], w=[pT])
                    if k % 4 == 3:
                        yield
                kb.op("act", lambda g: g.copy(out=dst[:, 0:nk * 128], in_=pT[:, 0:nk * 128]), r=[pT], w=[dst])
                yield

            def proj(srcT, w, nk, evac):
                for a0 in (0, 512):
                    for k in range(nk):
                        kb.op("pe", lambda g: g.matmul(pF[:, 0:512], lhsT=srcT[:, k * 128:(k + 1) * 128],
                                                       rhs=w[:, k, a0:a0 + 512], start=(k == 0), stop=(k == nk - 1)),
                              r=[srcT, w], w=[pF])
                        if k % 4 == 3:
                            yield
                    evac(a0)
                    yield

            yield from transposes(ya, yaT, 4)
            yield from proj(yaT, w_mo, 4, lambda a0: kb.op("dve", lambda g: g.tensor_tensor(
                out=m1[:, a0:a0 + 512], in0=pF[:, 0:512], in1=za[:, a0:a0 + 512], op=ALU.mult), r=[pF, za], pw=[m1]))
            yield from transposes(yr, yrT, 8)
            yield from proj(yrT, w_ro, 8, lambda a0: kb.op("dve", lambda g: g.tensor_tensor(
                out=m2[:, a0:a0 + 512], in0=pF[:, 0:512], in1=zr[:, a0:a0 + 512], op=ALU.mult), r=[pF, zr], pw=[m2]))
            kb.op("pool", lambda g: g.tensor_tensor(out=mg[:], in0=m1[:], in1=m2[:], op=ALU.add), r=[m1, m2], w=[mg])
            yield
            yield from transposes(mg, mgT, 8)
            yield from proj(mgT, w_xo, 8, lambda a0: kb.op("dve", lambda g: g.tensor_tensor(
                out=hh[:, a0:a0 + 512], in0=pF[:, 0:512], in1=xt[:, a0:a0 + 512], op=ALU.add), r=[pF, xt], pw=[hh]))
            kb.op("act", lambda g: g.activation(out=junkr[:], in_=hh[:], func=AF.Square, accum_out=ss[:, 0:1]),
                  r=[hh], w=[junkr, ss])
            rstd_from_ss(kb, (ss[:, 0:1], ss), (rs[:, 0:1], rs), epsc, 1024.0)
            kb.op("dve", lambda g: g.scalar_tensor_tensor(out=xn[:], in0=hh[:], scalar=rs[:, 0:1], in1=gffn[:],
                                                          op0=ALU.mult, op1=ALU.mult), r=[hh, rs, gffn], w=[xn])
            yield
            yield from transposes(xn, xnT, 8)
            for hb in range(2):
                for h in range(4 * hb, 4 * hb + 4):
                    for k in range(8):
                        kb.op("pe", lambda g: g.matmul(pF[:, (h % 4) * 128:(h % 4 + 1) * 128],
                                                       lhsT=w_pq[:, k, h * 128:(h + 1) * 128],
                                                       rhs=xnT[:, k * 128:(k + 1) * 128], start=(k == 0), stop=(k == 7)),
                              r=[w_pq, xnT], w=[pF])
                        if k % 4 == 3:
                            yield
                kb.op("act", lambda g: g.copy(out=pqT[:, hb * 512:(hb + 1) * 512], in_=pF[:, 0:512]), r=[pF], pw=[pqT])
                yield
            for hp in range(4):
                for h in (2 * hp, 2 * hp + 1):
                    kb.op("pe", lambda g: g.matmul(pF[:, (h % 2) * 256:(h % 2 + 1) * 256], lhsT=pqT[:, h * 128:(h + 1) * 128],
                                                   rhs=keys[:], start=True, stop=True), r=[pqT, keys], w=[pF])
                kb.op("act", lambda g: g.copy(out=sc[:, 2 * hp:2 * hp + 2, :].rearrange("p h k -> p (h k)"), in_=pF[:, 0:512]),
                      r=[pF], pw=[sc])
                yield
            for h in range(8):
                for hf in range(2):
                    src = sc[:, h, hf * 128:(hf + 1) * 128]
                    srm = scm[:, h, hf * 128:(hf + 1) * 128]
                    kb.op("dve", lambda g: g.max(out=v12[:, h, hf, 0:8], in_=src), r=[sc], pw=[v12])
                    kb.op("dve", lambda g: g.max_index(out=i12[:, h, hf, 0:8], in_max=v12[:, h, hf, 0:8], in_values=src),
                          r=[sc, v12], pw=[i12])
                    kb.op("dve", lambda g: g.match_replace(out=srm, in_to_replace=v12[:, h, hf, 0:8], in_values=src,
                                                           imm_value=NEG), r=[sc, v12], pw=[scm])
                    yield
                    kb.op("dve", lambda g: g.max(out=v12[:, h, hf, 8:16], in_=srm), r=[scm], pw=[v12])
                    kb.op("dve", lambda g: g.max_index(out=i12[:, h, hf, 8:16], in_max=v12[:, h, hf, 8:16],
                                                       in_values=srm), r=[scm, v12], pw=[i12])
                    yield
            kb.op("dve", lambda g: g.tensor_tensor(out=cand[:].rearrange("p h (a b) -> p h a b", a=16),
                                                   in0=v12[:, :, 0, :].unsqueeze(3).to_broadcast([128, 8, 16, 16]),
                                                   in1=v12[:, :, 1, :].unsqueeze(2).to_broadcast([128, 8, 16, 16]),
                                                   op=ALU.add), r=[v12], w=[cand])
            yield
            for h in range(8):
                kb.op("dve", lambda g: g.max(out=cv[:, h, 0:8], in_=cand[:, h, :]), r=[cand], pw=[cv])
                kb.op("dve", lambda g: g.max_index(out=ci[:, h, 0:8], in_max=cv[:, h, 0:8], in_values=cand[:, h, :]),
                      r=[cand, cv], pw=[ci])
                kb.op("dve", lambda g: g.match_replace(out=candm[:, h, :], in_to_replace=cv[:, h, 0:8],
                                                       in_values=cand[:, h, :], imm_value=NEG), r=[cand, cv], pw=[candm])
                yield
                kb.op("dve", lambda g: g.max(out=cv[:, h, 8:16], in_=candm[:, h, :]), r=[candm], pw=[cv])
                kb.op("dve", lambda g: g.max_index(out=ci[:, h, 8:16], in_max=cv[:, h, 8:16], in_values=candm[:, h, :]),
                      r=[candm, cv], pw=[ci])
                yield
            kb.op("dve", lambda g: g.tensor_tensor(out=es_[:], in0=cv[:],
                                                   in1=cv[:, :, 0:1].to_broadcast([128, 8, 16]), op=ALU.subtract),
                  r=[cv], w=[es_])
            kb.op("act", lambda g: g.activation(out=es_[:], in_=es_[:], func=AF.Exp), r=[es_], w=[es_])
            yield
            kb.op("dve", lambda g: g.tensor_reduce(out=sm8[:], in_=es_[:], axis=AX.X, op=ALU.add), r=[es_], w=[sm8])
            kb.op("dve", lambda g: g.reciprocal(out=sm8[:], in_=sm8[:]), r=[sm8], w=[sm8])
            kb.op("dve", lambda g: g.tensor_tensor(out=gts[:].rearrange("p (h k) -> p h k", h=8), in0=es_[:],
                                                   in1=sm8[:].unsqueeze(2).to_broadcast([128, 8, 16]), op=ALU.mult),
                  r=[es_, sm8], w=[gts])
            yield
            kb.op("dve", lambda g: g.tensor_copy(out=cf[:], in_=ci[:]), r=[ci], w=[cf])
            kb.op("dve", lambda g: g.tensor_copy(out=i12f[:], in_=i12[:]), r=[i12], w=[i12f])
            yield
            kb.op("dve", lambda g: g.tensor_tensor(out=oh[:], in0=cf[:].unsqueeze(3).to_broadcast([128, 8, 16, 16]),
                                                   in1=iota[:, 16:32].unsqueeze(1).unsqueeze(1).to_broadcast([128, 8, 16, 16]),
                                                   op=ALU.is_ge), r=[cf, iota], w=[oh])
            yield
            kb.op("dve", lambda g: g.tensor_reduce(out=af[:], in_=oh[:], axis=AX.X, op=ALU.add), r=[oh], w=[af])
            kb.op("dve", lambda g: g.scalar_tensor_tensor(out=bf[:], in0=af[:], scalar=-16.0, in1=cf[:], op0=ALU.mult,
                                                          op1=ALU.add), r=[af, cf], w=[bf])
            yield
            for (sel_, hf, dst) in ((af, 0, e1b), (bf, 1, e2b)):
                kb.op("dve", lambda g: g.tensor_tensor(out=oh[:], in0=sel_[:].unsqueeze(3).to_broadcast([128, 8, 16, 16]),
                                                       in1=iota[:, 0:16].unsqueeze(1).unsqueeze(1).to_broadcast([128, 8, 16, 16]),
                                                       op=ALU.is_equal), r=[sel_, iota], w=[oh])
                yield
                kb.op("dve", lambda g: g.tensor_tensor(out=oh[:], in0=oh[:],
                                                       in1=i12f[:, :, hf, :].unsqueeze(2).to_broadcast([128, 8, 16, 16]),
                                                       op=ALU.mult), r=[oh, i12f], w=[oh])
                yield
                with kb.nc.allow_low_precision("one-hot select of an integer < 128: exact in bf16"):
                    kb.op("dve", lambda g: g.tensor_reduce(out=dst[:].rearrange("p (h k) -> p h k", h=8), in_=oh[:],
                                                           axis=AX.X, op=ALU.add), r=[oh], w=[dst])
                yield
            kb.op("dve", lambda g: g.tensor_copy(out=ghi[:], in_=gts[:]), r=[gts], w=[ghi])
            kb.op("dve", lambda g: g.tensor_tensor(out=glo[:], in0=gts[:], in1=ghi[:], op=ALU.subtract), r=[gts, ghi], w=[glo])
            for i_, src_ in enumerate((e1b, e2b, ghi, glo)):
                kb.op("pe", lambda g: g.transpose(out=pT[:, i_ * 128:(i_ + 1) * 128], in_=src_[:], identity=ident[:]),
                      r=[src_, ident], w=[pT])
            kb.op("act", lambda g: g.copy(out=tsb[:], in_=pT[:, 0:512]), r=[pT], w=[tsb])
            yield
            kb.op("dve", lambda g: g.scalar_tensor_tensor(out=ef[:], in0=tsb[:, 0:128], scalar=128.0, in1=tsb[:, 128:256],
                                                          op0=ALU.mult, op1=ALU.add), r=[tsb], w=[ef])
            kb.op("dve", lambda g: g.tensor_copy(out=eT[:], in_=ef[:]), r=[ef], w=[eT])
            kb.op("dve", lambda g: g.tensor_tensor(out=gT[:], in0=tsb[:, 256:384], in1=tsb[:, 384:512], op=ALU.add),
                  r=[tsb], w=[gT])
            yield

        def experts(T, nxt):
            r0 = T * 128
            hh, xn, eT, gT = hh2[T % 2], xn2[T % 2], eT2[T % 2], gT2[T % 2]
            pBB = [pB, pA]

            def s_sel(t):
                sl = selb[t % NG]
                kb.op("act", lambda g: g.copy(out=sl[:], in_=ident[:, t:t + 1].to_broadcast([128, 128])),
                      r=[ident], w=[sl])

            def s0(t):
                uv = uvs[t % NG]
                kb.dma("pool", None, None, r=[eT, D["UVB"]], w=[uv],
                       fn=lambda g: g.indirect_dma_start(out=uv[:], out_offset=None, in_=D["UVB"][:, :],
                                                         in_offset=bass.IndirectOffsetOnAxis(ap=eT[:, t:t + 1], axis=0)))

            def s0b(t):
                sl = selb[t % NG]
                pb = pBB[t % 2]
                for (a0, a1) in ((0, 512), (512, 1024)):
                    kb.op("pe", lambda g: g.matmul(pb[:, a0:a1], lhsT=sl[:], rhs=xn[:, a0:a1], start=True, stop=True),
                          r=[sl, xn], w=[pb])

            def s1(t):
                uv = uvs[t % NG]
                pb = pBB[t % 2]
                ac = acol[t % NG]
                wc = wcol[t % NG]
                kb.op("dve", lambda g: g.scalar_tensor_tensor(out=junk[:], in0=uv[:, 0:1024], scalar=1.0, in1=pb[:],
                                                              op0=ALU.mult, op1=ALU.mult, accum_out=ac[:, 0:1]),
                      r=[uv, pb], w=[junk, ac])
                kb.op("act", lambda g: g.activation(out=wc[:, 0:1], in_=ac[:, 0:1], func=AF.Gelu), r=[ac], w=[wc])

            def s2(t):
                uv = uvs[t % NG]
                g_ = G[t % NG]
                wc = wcol[t % NG]
                kb.op("dve", lambda g: g.tensor_scalar(out=g_[:], in0=zc[:, 127 - t:255 - t], scalar1=wc[:, 0:1],
                                                       scalar2=gT[:, t:t + 1], op0=ALU.mult, op1=ALU.mult),
                      r=[zc, wc, gT], w=[g_])
                for (a0, a1) in ((0, 512), (512, 1024)):
                    kb.op("pe", lambda g: g.matmul(pC[:, a0:a1], lhsT=g_[:], rhs=uv[:, 1024 + a0:1024 + a1], start=(t == 0),
                                                   stop=(t == 127)), r=[g_, uv], w=[pC])

            for j in range(4):
                s_sel(j)
            for i in range(128 + 5):
                if i + 4 < 128:
                    s_sel(i + 4)
                if i < 128:
                    s0(i)
                if 0 <= i - 2 < 128:
                    s0b(i - 2)
                if 0 <= i - 3 < 128:
                    s1(i - 3)
                if nxt is not None:
                    next(nxt, None)
                    next(nxt, None)
                if 0 <= i - 5 < 128:
                    s2(i - 5)
            kb.op("dve", lambda g: g.tensor_tensor(out=ot[:], in0=pC[:], in1=hh[:], op=ALU.add), r=[pC, hh], w=[ot])
            kb.dma("sp", D["out"][r0 - 128:r0, :], ot[:], r=[ot], pw=[D["out"]])

        tiles = list(tiles)
        for _ in route(tiles[0]):
            pass
        for i, T in enumerate(tiles):
            nxt = route(tiles[i + 1]) if i + 1 < len(tiles) else None
            experts(T, nxt)
            if nxt is not None:
                for _ in nxt:
                    pass
        kb.barrier()


def host_constants():
    c = {}
    pos = (np.arange(LP) - 112).astype(np.float32)

    def tab(half):
        inv = (10000.0 ** (-np.arange(half, dtype=np.float32) / half)).astype(np.float32)
        ang = (pos[:, None] * inv[None, :]).astype(np.float32)
        lay = lambda a: np.ascontiguousarray(a.reshape(NT, 128, half).transpose(1, 0, 2).reshape(128, NT * half))
        return lay(np.cos(ang).astype(np.float32)), lay(np.sin(ang).astype(np.float32))

    c["cosm"], c["sinm"] = tab(16)
    c["cosr"], c["sinr"] = tab(32)
    H, C = 8, 128
    lg = np.log(1.0 - 2.0 ** (-5.0 - np.arange(H, dtype=np.float32))).astype(np.float32)
    idx = np.arange(C, dtype=np.float32)
    diff = idx[:, None] - idx[None, :]
    decay = np.where(diff[None] >= 0, np.exp(np.maximum(diff, 0.0)[None] * lg[:, None, None]), 0.0)
    c["decT"] = np.ascontiguousarray(decay.transpose(0, 2, 1) * 0.125).astype(np.float32)
    zeta = np.exp((C - 1 - idx)[None, :] * lg[:, None]) * 0.125
    c["zeta"] = np.ascontiguousarray(zeta.T).astype(np.float32)
    xi = np.exp((idx + 1)[None, :] * lg[:, None])
    c["xi"] = np.ascontiguousarray(np.broadcast_to(xi[:, None, :], (H, 64, C))).astype(np.float32)
    c["gamma"] = np.exp(C * lg).astype(np.float32)
    c["ident"] = np.eye(128, dtype=np.float32)
    k = np.arange(128)
    c["tri"] = (k[:, None] <= k[None, :]).astype(np.float32)
    zc = np.zeros((128, 255), np.float32)
    zc[:, 127] = 1.0
    c["zc"] = zc
    io = np.concatenate([np.arange(16, dtype=np.float32), 16.0 * np.arange(1, 17, dtype=np.float32)])
    c["iota16"] = np.ascontiguousarray(np.broadcast_to(io[None], (128, 32)))
    c["epsc"] = np.full((128, 1), EPS, np.float32)
    return c


SCRATCH = {"QT": ([8, 96, LP], BF16), "KT": ([8, 96, LP], BF16), "VA": ([LP, 520], BF16),
           "RQT": ([8, 64, LP], BF16), "RKT": ([8, 64, LP], BF16), "RK": ([LP, 512], BF16),
           "RV": ([LP, 1024], BF16), "RG": ([LP, 1024], BF16), "ZA": ([LP, 1024], BF16), "ZR": ([LP, 1024], BF16),
           "YA": ([4096, 512], BF16), "YR": ([LP, 1024], BF16),
           "UVB": ([16384, 2048], BF16)}

INPUTS = {"xpad": [LP, 1024], "w_in": [1024, 5792], "w_uq": [384, 768], "w_ukv": [256, 1024],
          "w_mla_out": [512, 1024], "w_ret_out": [1024, 1024], "w_mix_out": [1024, 1024], "w_peer_q": [1024, 1024],
          "peer_u": [16384, 1024], "peer_v": [16384, 1024], "keysbd": [128, 256],
          "g_mix": [128, 1024], "g_q_lora": [128, 384], "g_kv_lora": [128, 256], "g_qk_q": [128, 96],
          "g_qk_k": [128, 96], "g_ret_gn": [128, 1024], "g_ffn": [128, 1024],
          "cosm": [128, NT * 16], "sinm": [128, NT * 16], "cosr": [128, NT * 32], "sinr": [128, NT * 32],
          "decT": [8, 128, 128], "zeta": [128, 8], "xi": [8, 64, 128], "ident": [128, 128], "tri": [128, 128],
          "zc": [128, 255], "iota16": [128, 32], "epsc": [128, 1]}


def build(phases="abcd", debug=False, tiles=None, ext_in=(), stop=0):
    kb = KB()
    D = {}
    for n, shp in INPUTS.items():
        D[n] = kb.dram(n, shp, F32, "ExternalInput")
    for n, (shp, dt) in SCRATCH.items():
        D[n] = kb.dram(n, shp, dt, "ExternalInput" if n in ext_in else ("ExternalOutput" if debug else "Internal"))
    D["out"] = kb.dram("out", [4096, 1024], F32, "ExternalOutput")
    if debug:
        D["DBG"] = kb.dram("DBG", [128, 512], F32, "ExternalOutput")
    gam = host_constants()["gamma"]
    if "a" in phases:
        phase_a(kb, D)
    if "b" in phases:
        phase_b(kb, D)
    if "c" in phases:
        phase_c(kb, D, gam)
    if "d" in phases:
        phase_d(kb, D, tiles if tiles is not None else list(range(1, NT)), stop)
    kb.barrier()
    return kb


def make_in_maps(inputs):
    c = host_constants()
    f = lambda a: np.ascontiguousarray(np.asarray(a, dtype=np.float32))
    rep = lambda v: np.ascontiguousarray(np.broadcast_to(f(v).reshape(1, -1), (128, f(v).size)))
    shared = {
        "w_in": f(inputs["w_in"][0]), "w_uq": f(inputs["w_uq"][0]), "w_ukv": f(inputs["w_ukv"][0]),
        "w_mla_out": f(inputs["w_mla_out"][0]), "w_ret_out": f(inputs["w_ret_out"][0]),
        "w_mix_out": f(inputs["w_mix_out"][0]), "w_peer_q": f(inputs["w_peer_q"][0]),
        "peer_u": f(inputs["peer_u"][0]), "peer_v": f(inputs["peer_v"][0]),
        "g_mix": rep(inputs["g_mix"][0]), "g_q_lora": rep(inputs["g_q_lora"][0]),
        "g_kv_lora": rep(inputs["g_kv_lora"][0]), "g_qk_q": rep(inputs["g_qk_q"][0]),
        "g_qk_k": rep(inputs["g_qk_k"][0]), "g_ret_gn": rep(inputs["g_ret_gn"][0]), "g_ffn": rep(inputs["g_ffn"][0]),
    }
    kbd = np.zeros((128, 256), np.float32)
    kbd[0:64, 0:128] = f(inputs["peer_keys_1"][0]).T
    kbd[64:128, 128:256] = f(inputs["peer_keys_2"][0]).T
    shared["keysbd"] = kbd
    for n in ("cosm", "sinm", "cosr", "sinr", "decT", "zeta", "xi", "ident", "tri", "zc", "iota16", "epsc"):
        shared[n] = c[n]
    x = f(inputs["x"])
    meta = f(inputs["meta_tokens"])
    maps = []
    for b in range(x.shape[0]):
        xp = np.zeros((LP, 1024), np.float32)
        xp[112:128] = meta
        xp[128:] = x[b]
        m = dict(shared)
        m["xpad"] = xp
        maps.append(m)
    return maps


def kernel(**inputs):
    kb = build("abcd")
    maps = make_in_maps(inputs)
    res = run_bass_kernel_spmd(kb.nc, maps, core_ids=list(range(8)))
    return np.stack([np.asarray(r["out"], dtype=np.float32) for r in res.results], axis=0)
```

```python
import math
import numpy as np
from contextlib import ExitStack
import concourse.bass as bass
import concourse.mybir as mybir
from concourse.bass_utils import run_bass_kernel_spmd

F32 = mybir.dt.float32
BF16 = mybir.dt.bfloat16
I32 = mybir.dt.int32
U32 = mybir.dt.uint32
AF = mybir.ActivationFunctionType
ALU = mybir.AluOpType
AX = mybir.AxisListType

NT = 33
LP = NT * 128
EPS = 1e-6
NEG = -1.0e30
GROUPS = [(0, 384), (384, 672), (672, 1184), (1184, 1696), (1696, 2208), (2208, 2720),
          (2720, 3232), (3232, 3744), (3744, 4256), (4256, 4768), (4768, 5280), (5280, 5792)]


class Tl:
    def __init__(self, t):
        self.t = t
        self.w = {}
        self.r = {}

    def __getitem__(self, k):
        return self.t[k]


class KB:
    def __init__(self):
        nc = bass.Bass("TRN2", target_bir_lowering=False)
        self.nc = nc
        self.es = ExitStack()
        self.E = dict(pe=nc.tensor, dve=nc.vector, act=nc.scalar, pool=nc.gpsimd, sp=nc.sync)
        self.csem = {e: self.es.enter_context(nc.semaphore("c_" + e)) for e in ("pe", "dve", "act", "pool")}
        self.ccnt = {e: 0 for e in self.csem}
        self.NDS = 8
        self.dsem = {q: [self.es.enter_context(nc.semaphore("d_%s%d" % (q, i))) for i in range(self.NDS)]
                     for q in ("sp", "pool")}
        self.dcnt = {q: [0] * self.NDS for q in ("sp", "pool")}
        self.dnext = {q: 0 for q in ("sp", "pool")}
        self.waited = {e: {} for e in self.E}
        self.nwaits = 0
        self.ninst = 0

    def sb(self, es, name, shape, dt):
        return Tl(es.enter_context(self.nc.sbuf_tensor("s_" + name, shape, dt)))

    def ps(self, es, name, shape, dt):
        return Tl(es.enter_context(self.nc.psum_tensor("p_" + name, shape, dt)))

    def dram(self, name, shape, dt, kind):
        return Tl(self.nc.dram_tensor(name, shape, dt, kind=kind).ap())

    def _deps(self, r, w, pw):
        deps = []
        for b in r:
            deps.extend(b.w.values())
        for b in w:
            deps.extend(b.w.values())
            deps.extend(b.r.values())
        for b in pw:
            deps.extend(b.r.values())
        return deps

    def _wait(self, e, deps):
        best = {}
        for (key, sem, val, src) in deps:
            if src == e and e == "pe":
                continue
            if key not in best or best[key][1] < val:
                best[key] = (sem, val)
        for key, (sem, val) in best.items():
            if self.waited[e].get(key, 0) >= val:
                continue
            self.E[e].wait_ge(sem, val)
            self.waited[e][key] = val
            self.nwaits += 1

    def _commit(self, ev, r, w, pw):
        key = ev[0]
        for b in w:
            b.w = {key: ev}
            b.r = {}
        for b in pw:
            b.w[key] = ev
        for b in r:
            if not any(b is x for x in w) and not any(b is x for x in pw):
                b.r[key] = ev

    def op(self, e, fn, r=(), w=(), pw=()):
        self._wait(e, self._deps(r, w, pw))
        inst = fn(self.E[e])
        self.ccnt[e] += 1
        inst.then_inc(self.csem[e], 1)
        ev = ("c_" + e, self.csem[e], self.ccnt[e], e)
        self._commit(ev, r, w, pw)
        self.ninst += 1
        return ev

    def dma(self, q, out, in_, r=(), w=(), pw=(), fn=None):
        deps = self._deps(r, w, pw)
        i = self.dnext[q]
        self.dnext[q] = (i + 1) % self.NDS
        sem = self.dsem[q][i]
        key = "d_%s%d" % (q, i)
        if self.dcnt[q][i] > 0:
            deps.append((key, sem, self.dcnt[q][i], "dma"))
        self._wait(q, deps)
        if fn is None:
            inst = self.E[q].dma_start(out=out, in_=in_)
        else:
            inst = fn(self.E[q])
        self.dcnt[q][i] += 16
        inst.then_inc(sem, 16)
        ev = (key, sem, self.dcnt[q][i], "dma")
        self._commit(ev, r, w, pw)
        self.ninst += 1
        return ev

    def all_events(self):
        evs = []
        for e in self.csem:
            if self.ccnt[e] > 0:
                evs.append(("c_" + e, self.csem[e], self.ccnt[e], e))
        for q in self.dsem:
            for i in range(self.NDS):
                if self.dcnt[q][i] > 0:
                    evs.append(("d_%s%d" % (q, i), self.dsem[q][i], self.dcnt[q][i], "dma"))
        return evs

    def barrier(self):
        evs = self.all_events()
        for e in self.E:
            self._wait(e, [ev for ev in evs if not (ev[3] == e and e == "pe")])


def tt(kb, e, out, a, b, op, r, w, pw=()):
    return kb.op(e, lambda g: g.tensor_tensor(out=out, in0=a, in1=b, op=op), r=r, w=w, pw=pw)


def rope(kb, e, o1, o2, x1, x2, cos, sin, ta, tb, rd, wr):
    A, B = ta[1], tb[1]
    tt(kb, e, A, x1, cos, ALU.mult, rd, [ta[0]])
    tt(kb, e, B, x2, sin, ALU.mult, rd, [tb[0]])
    tt(kb, e, o1, A, B, ALU.subtract, [ta[0], tb[0]], [], pw=wr)
    tt(kb, e, A, x1, sin, ALU.mult, rd, [ta[0]])
    tt(kb, e, B, x2, cos, ALU.mult, rd, [tb[0]])
    tt(kb, e, o2, A, B, ALU.add, [ta[0], tb[0]], [], pw=wr)


def rstd_from_ss(kb, ss, rs, epsc, n):
    kb.op("act", lambda g: g.activation(out=rs[0], in_=ss[0], func=AF.Sqrt, bias=epsc[:, 0:1], scale=1.0 / n),
          r=[ss[1], epsc], w=[rs[1]])
    kb.op("dve", lambda g: g.reciprocal(out=rs[0], in_=rs[0]), r=[rs[1]], w=[rs[1]])


def phase_a(kb, D):
    with ExitStack() as es:
        sb = lambda n, s, d: kb.sb(es, n, s, d)
        ps = lambda n, s, d: kb.ps(es, n, s, d)
        w_in = sb("w_in", [128, 8, 5792], BF16)
        w_uq = sb("w_uq", [128, 3, 768], BF16)
        w_ukv = sb("w_ukv", [128, 2, 1024], BF16)
        ident = sb("identA", [128, 128], BF16)
        gmix = sb("gmix", [128, 1024], F32)
        gql = sb("gql", [128, 384], F32)
        gkvl = sb("gkvl", [128, 256], F32)
        gq = sb("gq", [128, 96], F32)
        gk = sb("gk", [128, 96], F32)
        cosm = sb("cosm", [128, NT, 16], F32)
        sinm = sb("sinm", [128, NT, 16], F32)
        cosr = sb("cosr", [128, NT, 32], F32)
        sinr = sb("sinr", [128, NT, 32], F32)
        epsc = sb("epscA", [128, 1], F32)
        xt = [sb("xt%d" % i, [128, 1024], F32) for i in range(2)]
        junk = sb("junkA", [128, 1024], BF16)
        ss = sb("ssA", [128, 1], F32)
        rs = sb("rsA", [128, 1], F32)
        hn = sb("hn", [128, 1024], BF16)
        hnT = sb("hnT", [128, 1024], BF16)
        cqn = sb("cqn", [128, 384], BF16)
        cqT = sb("cqT", [128, 384], BF16)
        ckvn = sb("ckvn", [128, 256], BF16)
        ckvT = sb("ckvT", [128, 256], BF16)
        sq = sb("sqA", [128, 1024], F32)
        st8 = sb("st8", [128, 8], F32)
        rs8 = sb("rs8", [128, 8], F32)
        ss1 = sb("ss1", [128, 1], F32)
        qn = sb("qn", [128, 8, 96], F32)
        qf = sb("qf", [128, 8, 96], BF16)
        kf = sb("kf", [128, 8, 96], BF16)
        kpg = sb("kpg", [128, 32], F32)
        rk = sb("rkA", [128, 32], F32)
        ta = sb("taA", [128, 8, 32], F32)
        tb = sb("tbA", [128, 8, 32], F32)
        tc = sb("tcA", [128, 8, 32], F32)
        td = sb("tdA", [128, 8, 32], F32)
        qT = sb("qTsb", [96, 8, 128], BF16)
        kT = sb("kTsb", [96, 8, 128], BF16)
        va = sb("vaA", [128, 8, 65], BF16)
        rqf = sb("rqf", [128, 8, 64], BF16)
        rkf = sb("rkf", [128, 8, 64], BF16)
        rqT = sb("rqTsb", [128, 4, 128], BF16)
        rkT = sb("rkTsb", [128, 4, 128], BF16)
        ob = [sb("obA%d" % i, [128, 512], BF16) for i in range(4)]
        zp = [ps("zp%d" % i, [128, 512], F32) for i in range(3)]
        tpa = ps("tpa", [128, 1024], BF16)
        tpb = ps("tpb", [128, 1024], BF16)
        qp = ps("qp", [128, 1024], F32)

        for k in range(8):
            for c0, c1 in ((0, 2048), (2048, 4096), (4096, 5792)):
                kb.dma("pool", w_in[:, k, c0:c1], D["w_in"][k * 128:(k + 1) * 128, c0:c1], pw=[w_in])
        for k in range(3):
            kb.dma("pool", w_uq[:, k, :], D["w_uq"][k * 128:(k + 1) * 128, :], pw=[w_uq])
        for k in range(2):
            kb.dma("pool", w_ukv[:, k, :], D["w_ukv"][k * 128:(k + 1) * 128, :], pw=[w_ukv])
        kb.dma("pool", ident[:], D["ident"][:, :], w=[ident])
        for t_, n_ in ((gmix, "g_mix"), (gql, "g_q_lora"), (gkvl, "g_kv_lora"), (gq, "g_qk_q"), (gk, "g_qk_k"),
                       (epsc, "epsc")):
            kb.dma("sp", t_[:], D[n_][:, :], w=[t_])
        for t_, n_ in ((cosm, "cosm"), (sinm, "sinm"), (cosr, "cosr"), (sinr, "sinr")):
            kb.dma("sp", t_[:].rearrange("p t d -> p (t d)"), D[n_][:, :], w=[t_])
        kb.op("pool", lambda g: g.memset(va[:].rearrange("p h d -> p (h d)"), 1.0), w=[va])

        sq1 = sb("sq1A", [128, 1024], F32)
        ss1b = sb("ss1bA", [128, 1], F32)
        ss2 = sb("ss2A", [128, 1], F32)
        st8b = sb("st8bA", [128, 8], F32)
        rs8b = sb("rs8bA", [128, 8], F32)
        qn1 = sb("qn1A", [128, 8, 96], F32)
        ta1 = sb("ta1A", [128, 8, 32], F32)
        tb1 = sb("tb1A", [128, 8, 32], F32)

        def post0(z, T):
            kb.op("act", lambda g: g.activation(out=sq[:, 0:384], in_=z[:, 0:384], func=AF.Square,
                                                accum_out=ss1[:, 0:1]), r=[z], w=[sq, ss1])
            rstd_from_ss(kb, (ss1[:, 0:1], ss1), (ss1[:, 0:1], ss1), epsc, 384.0)
            kb.op("dve", lambda g: g.scalar_tensor_tensor(out=cqn[:], in0=z[:, 0:384], scalar=ss1[:, 0:1],
                                                          in1=gql[:], op0=ALU.mult, op1=ALU.mult),
                  r=[z, ss1, gql], w=[cqn])
            yield
            for k in range(3):
                kb.op("pe", lambda g: g.transpose(out=tpb[:, k * 128:(k + 1) * 128],
                                                  in_=cqn[:, k * 128:(k + 1) * 128], identity=ident[:]),
                      r=[cqn, ident], w=[tpb])
            kb.op("dve", lambda g: g.tensor_copy(out=cqT[:], in_=tpb[:, 0:384]), r=[tpb], w=[cqT])
            yield
            for (a0, a1) in ((0, 512), (512, 768)):
                for k in range(3):
                    kb.op("pe", lambda g: g.matmul(qp[:, a0:a1], lhsT=cqT[:, k * 128:(k + 1) * 128],
                                                   rhs=w_uq[:, k, a0:a1], start=(k == 0), stop=(k == 2)),
                          r=[cqT, w_uq], w=[qp])
            kb.op("act", lambda g: g.activation(out=sq[:, 0:768], in_=qp[:, 0:768], func=AF.Square),
                  r=[qp], w=[sq])
            kb.op("dve", lambda g: g.tensor_reduce(out=st8[:], in_=sq[:, 0:768].rearrange("p (h d) -> p h d", h=8),
                                                   axis=AX.X, op=ALU.add), r=[sq], w=[st8])
            rstd_from_ss(kb, (st8[:], st8), (rs8[:], rs8), epsc, 96.0)
            kb.op("dve", lambda g: g.tensor_tensor(out=qn[:], in0=qp[:, 0:768].rearrange("p (h d) -> p h d", h=8),
                                                   in1=rs8[:].unsqueeze(2).to_broadcast([128, 8, 96]),
                                                   op=ALU.mult), r=[qp, rs8], w=[qn])
            yield
            kb.op("pool", lambda g: g.tensor_tensor(out=qn[:], in0=qn[:],
                                                    in1=gq[:].unsqueeze(1).to_broadcast([128, 8, 96]),
                                                    op=ALU.mult), r=[qn, gq], w=[qn])
            kb.op("pool", lambda g: g.tensor_copy(out=qf[:, :, 0:64], in_=qn[:, :, 0:64]), r=[qn], pw=[qf])
            cs = cosm[:, T, :].unsqueeze(1).to_broadcast([128, 8, 16])
            sn = sinm[:, T, :].unsqueeze(1).to_broadcast([128, 8, 16])
            rope(kb, "pool", qf[:, :, 64:80], qf[:, :, 80:96], qn[:, :, 64:80], qn[:, :, 80:96], cs, sn,
                 (ta, ta[:, :, 0:16]), (tb, tb[:, :, 0:16]), [qn, cosm, sinm], [qf])
            yield
            yield
            for h in range(8):
                kb.op("pe", lambda g: g.transpose(out=tpb[0:96, h * 128:(h + 1) * 128], in_=qf[:, h, :],
                                                  identity=ident[:]), r=[qf, ident], w=[tpb])
            kb.op("act", lambda g: g.copy(out=qT[:].rearrange("p h t -> p (h t)"), in_=tpb[0:96, :]),
                  r=[tpb], w=[qT])
            kb.dma("sp", D["QT"][:, :, T * 128:(T + 1) * 128].rearrange("h d t -> d h t"), qT[:],
                   r=[qT], pw=[D["QT"]])
            yield

        def post1(z, T):
            kb.op("act", lambda g: g.activation(out=sq1[:, 0:256], in_=z[:, 0:256], func=AF.Square,
                                                accum_out=ss1b[:, 0:1]), r=[z], w=[sq1, ss1b])
            rstd_from_ss(kb, (ss1b[:, 0:1], ss1b), (ss1b[:, 0:1], ss1b), epsc, 256.0)
            kb.op("dve", lambda g: g.scalar_tensor_tensor(out=ckvn[:], in0=z[:, 0:256], scalar=ss1b[:, 0:1],
                                                          in1=gkvl[:], op0=ALU.mult, op1=ALU.mult),
                  r=[z, ss1b, gkvl], w=[ckvn])
            kb.op("act", lambda g: g.activation(out=sq1[:, 0:32], in_=z[:, 256:288], func=AF.Square,
                                                accum_out=ss2[:, 0:1]), r=[z], w=[sq1, ss2])
            kb.op("dve", lambda g: g.tensor_tensor(out=kpg[:], in0=z[:, 256:288], in1=gk[:, 64:96], op=ALU.mult),
                  r=[z, gk], w=[kpg])
            yield
            for k in range(2):
                kb.op("pe", lambda g: g.transpose(out=tpb[:, k * 128:(k + 1) * 128],
                                                  in_=ckvn[:, k * 128:(k + 1) * 128], identity=ident[:]),
                      r=[ckvn, ident], w=[tpb])
            kb.op("dve", lambda g: g.tensor_copy(out=ckvT[:], in_=tpb[:, 0:256]), r=[tpb], w=[ckvT])
            yield
            for (a0, a1) in ((0, 512), (512, 1024)):
                for k in range(2):
                    kb.op("pe", lambda g: g.matmul(qp[:, a0:a1], lhsT=ckvT[:, k * 128:(k + 1) * 128],
                                                   rhs=w_ukv[:, k, a0:a1], start=(k == 0), stop=(k == 1)),
                          r=[ckvT, w_ukv], w=[qp])
            kv3 = qp[:, 0:1024].rearrange("p (h d) -> p h d", h=8)
            kb.op("act", lambda g: g.activation(out=sq1[:, 0:512].rearrange("p (h d) -> p h d", h=8),
                                                in_=kv3[:, :, 0:64], func=AF.Square), r=[qp], w=[sq1])
            kb.op("dve", lambda g: g.tensor_reduce(out=st8b[:], in_=sq1[:, 0:512].rearrange("p (h d) -> p h d", h=8),
                                                   axis=AX.X, op=ALU.add), r=[sq1], w=[st8b])
            kb.op("dve", lambda g: g.tensor_scalar(out=st8b[:], in0=st8b[:], scalar1=ss2[:, 0:1], scalar2=None,
                                                   op0=ALU.add), r=[st8b, ss2], w=[st8b])
            rstd_from_ss(kb, (st8b[:], st8b), (rs8b[:], rs8b), epsc, 96.0)
            kb.op("dve", lambda g: g.tensor_tensor(out=qn1[:, :, 0:64], in0=kv3[:, :, 0:64],
                                                   in1=rs8b[:].unsqueeze(2).to_broadcast([128, 8, 64]),
                                                   op=ALU.mult), r=[qp, rs8b], w=[qn1])
            kb.op("act", lambda g: g.copy(out=va[:, :, 0:64], in_=kv3[:, :, 64:128]), r=[qp], w=[va])
            kb.dma("sp", D["VA"][T * 128:(T + 1) * 128, :], va[:].rearrange("p h d -> p (h d)"),
                   r=[va], pw=[D["VA"]])
            yield
            kb.op("pool", lambda g: g.tensor_tensor(out=kf[:, :, 0:64], in0=qn1[:, :, 0:64],
                                                    in1=gk[:, 0:64].unsqueeze(1).to_broadcast([128, 8, 64]),
                                                    op=ALU.mult), r=[qn1, gk], pw=[kf])
            rope(kb, "pool", rk[:, 0:16], rk[:, 16:32], kpg[:, 0:16], kpg[:, 16:32], cosm[:, T, :], sinm[:, T, :],
                 (ta1, ta1[:, 0, 0:16]), (tb1, tb1[:, 0, 0:16]), [kpg, cosm, sinm], [rk])
            kb.op("pool", lambda g: g.tensor_tensor(out=kf[:, :, 64:96],
                                                    in0=rk[:].unsqueeze(1).to_broadcast([128, 8, 32]),
                                                    in1=rs8b[:].unsqueeze(2).to_broadcast([128, 8, 32]),
                                                    op=ALU.mult), r=[rk, rs8b], pw=[kf])
            yield
            yield
            for h in range(8):
                kb.op("pe", lambda g: g.transpose(out=tpb[0:96, h * 128:(h + 1) * 128], in_=kf[:, h, :],
                                                  identity=ident[:]), r=[kf, ident], w=[tpb])
            kb.op("act", lambda g: g.copy(out=kT[:].rearrange("p h t -> p (h t)"), in_=tpb[0:96, :]),
                  r=[tpb], w=[kT])
            kb.dma("sp", D["KT"][:, :, T * 128:(T + 1) * 128].rearrange("h d t -> d h t"), kT[:],
                   r=[kT], pw=[D["KT"]])
            yield

        def post23(gi, z, T):
            dst = rqf if gi == 2 else rkf
            z3 = z[:, 0:512].rearrange("p (h d) -> p h d", h=8)
            cs = cosr[:, T, :].unsqueeze(1).to_broadcast([128, 8, 32])
            sn = sinr[:, T, :].unsqueeze(1).to_broadcast([128, 8, 32])
            rope(kb, "dve", dst[:, :, 0:32], dst[:, :, 32:64], z3[:, :, 0:32], z3[:, :, 32:64], cs, sn,
                 (tc, tc[:]), (td, td[:]), [z, cosr, sinr], [dst])
            yield
            yield
            dT = rqT if gi == 2 else rkT
            for j in range(4):
                kb.op("pe", lambda g: g.transpose(out=tpb[:, j * 128:(j + 1) * 128],
                                                  in_=dst[:, 2 * j:2 * j + 2, :].rearrange("p h d -> p (h d)"),
                                                  identity=ident[:]), r=[dst, ident], w=[tpb])
            kb.op("act", lambda g: g.copy(out=dT[:].rearrange("p j t -> p (j t)"), in_=tpb[:, 0:512]),
                  r=[tpb], w=[dT])
            name = "RQT" if gi == 2 else "RKT"
            kb.dma("sp", D[name][:, :, T * 128:(T + 1) * 128].rearrange("(j s) d t -> (s d) j t", s=2), dT[:],
                   r=[dT], pw=[D[name]])
            if gi == 3:
                kb.dma("sp", D["RK"][T * 128:(T + 1) * 128, :], dst[:].rearrange("p h d -> p (h d)"),
                       r=[dst], pw=[D["RK"]])
            yield

        pending = []

        for T in range(NT):
            x_ = xt[T % 2]
            kb.dma("sp", x_[:], D["xpad"][T * 128:(T + 1) * 128, :], w=[x_])
            kb.op("act", lambda g: g.activation(out=junk[:], in_=x_[:], func=AF.Square, accum_out=ss[:, 0:1]),
                  r=[x_], w=[junk, ss])
            rstd_from_ss(kb, (ss[:, 0:1], ss), (rs[:, 0:1], rs), epsc, 1024.0)
            kb.op("dve", lambda g: g.scalar_tensor_tensor(out=hn[:], in0=x_[:], scalar=rs[:, 0:1], in1=gmix[:],
                                                          op0=ALU.mult, op1=ALU.mult),
                  r=[x_, rs, gmix], w=[hn])
            for k in range(8):
                kb.op("pe", lambda g: g.transpose(out=tpa[:, k * 128:(k + 1) * 128], in_=hn[:, k * 128:(k + 1) * 128],
                                                  identity=ident[:]), r=[hn, ident], w=[tpa])
            kb.op("act", lambda g: g.copy(out=hnT[:], in_=tpa[:]), r=[tpa], w=[hnT])

            for gi, (c0, c1) in enumerate(GROUPS):
                n = c1 - c0
                z = zp[gi % 3]
                for k in range(8):
                    kb.op("pe", lambda g: g.matmul(z[:, 0:n], lhsT=hnT[:, k * 128:(k + 1) * 128], rhs=w_in[:, k, c0:c1],
                                                   start=(k == 0), stop=(k == 7)), r=[hnT, w_in], w=[z])
                if gi == 0:
                    pending.append(post0(z, T))
                elif gi == 1:
                    pending.append(post1(z, T))
                elif gi in (2, 3):
                    pending.append(post23(gi, z, T))
                else:
                    o = ob[gi % 4]
                    if gi in (4, 5):
                        kb.op("dve", lambda g: g.tensor_copy(out=o[:], in_=z[:, 0:512]), r=[z], w=[o])
                        name, cc = "RV", (gi - 4) * 512
                    elif gi in (6, 7):
                        kb.op("act", lambda g: g.activation(out=o[:], in_=z[:, 0:512], func=AF.Silu), r=[z], w=[o])
                        name, cc = "RG", (gi - 6) * 512
                    elif gi in (8, 9):
                        kb.op("act", lambda g: g.activation(out=o[:], in_=z[:, 0:512], func=AF.Sigmoid), r=[z], w=[o])
                        name, cc = "ZA", (gi - 8) * 512
                    else:
                        kb.op("act", lambda g: g.activation(out=o[:], in_=z[:, 0:512], func=AF.Sigmoid), r=[z], w=[o])
                        name, cc = "ZR", (gi - 10) * 512
                    kb.dma("sp", D[name][T * 128:(T + 1) * 128, cc:cc + 512], o[:], r=[o], pw=[D[name]])
                for gen in list(pending):
                    if next(gen, "done") == "done":
                        pending.remove(gen)
        for gen in pending:
            for _ in gen:
                pass
        kb.barrier()


def phase_b(kb, D):
    scale = 1.0 / math.sqrt(96.0)
    with ExitStack() as es:
        sb = lambda n, s, d: kb.sb(es, n, s, d)
        ps = lambda n, s, d: kb.ps(es, n, s, d)
        tri = sb("tri", [128, 128], BF16)
        kT = [sb("kTB%d" % i, [96, LP], BF16) for i in range(2)]
        qT = [sb("qTB%d" % i, [96, LP], BF16) for i in range(2)]
        vh = [sb("vhB%d" % i, [128, NT, 65], BF16) for i in range(2)]
        vm = [sb("vmB%d" % i, [16, 65], BF16) for i in range(2)]
        pT = [sb("pT%d" % i, [128, 512], BF16) for i in range(4)]
        ya = sb("yaB", [128, 32, 512], BF16)
        rc = sb("rcB", [128, 1], F32)
        sp_ = [ps("spB%d" % i, [128, 512], F32) for i in range(4)]
        op_ = [ps("opB%d" % i, [128, 512], F32) for i in range(4)]
        kb.dma("pool", tri[:], D["tri"][:, :], w=[tri])
        VAr = D["VA"].t.rearrange("(t p) c -> p t c", p=128)
        cvb = [sb("cvb%d" % i, [128, 4096], BF16) for i in range(2)]
        conv = [(src, off, c) for (src, off) in (("peer_u", 0), ("peer_v", 1024)) for c in range(32)]

        def convert(n):
            for _ in range(n):
                if not conv:
                    return
                src, off, c = conv.pop(0)
                cb = cvb[c % 2]
                sv = D[src].t.rearrange("(c p r) d -> c p (r d)", p=128, r=4)
                dv = D["UVB"].t.rearrange("(c p r) d -> c p r d", p=128, r=4)
                for a0 in (0, 2048):
                    kb.dma("pool", cb[:, a0:a0 + 2048], sv[c, :, a0:a0 + 2048], pw=[cb])
                kb.dma("sp", dv[c, :, :, off:off + 1024], cb[:].rearrange("p (r d) -> p r d", r=4), r=[cb], pw=[D["UVB"]])
        def head_loads(h):
            b = h % 2
            kb.dma("sp", kT[b][:], D["KT"][h, :, :], r=[D["KT"]], w=[kT[b]])
            kb.dma("sp", qT[b][:], D["QT"][h, :, :], r=[D["QT"]], w=[qT[b]])
            for t0 in range(0, NT, 11):
                kb.dma("sp", vh[b][:, t0:t0 + 11, :], VAr[:, t0:t0 + 11, h * 65:(h + 1) * 65], r=[D["VA"]], pw=[vh[b]])
            kb.dma("sp", vm[b][:], D["VA"][112:128, h * 65:(h + 1) * 65], r=[D["VA"]], w=[vm[b]])

        its = [(h, Q, kt) for h in range(8) for Q in range(8) for kt in range(0, 4 * Q + 5)]

        def geom(n):
            h, Q, kt = its[n]
            j0 = max(0, kt - (4 * Q + 1))
            return h, Q, kt, h % 2, (4 * Q + 1) * 128, j0, 128 * j0, (16 if kt == 0 else 128), sp_[n % 4], pT[n % 4]

        def emit_s(n):
            h, Q, kt, b, qc0, j0, c0, nk, s_, p_ = geom(n)
            if Q == 0 and kt == 0:
                head_loads(h)
            lhs = kT[b][:, 112:128] if kt == 0 else kT[b][:, kt * 128:(kt + 1) * 128]
            kb.op("pe", lambda g: g.matmul(s_[0:nk, c0:512], lhsT=lhs, rhs=qT[b][:, qc0 + c0:qc0 + 512],
                                           start=True, stop=True), r=[kT[b], qT[b]], w=[s_])

        def emit_rest(n):
            h, Q, kt, b, qc0, j0, c0, nk, s_, p_ = geom(n)
            if kt == 0:
                convert(1)
            kb.op("act", lambda g: g.activation(out=p_[0:nk, c0:512], in_=s_[0:nk, c0:512], func=AF.Exp,
                                                scale=scale), r=[s_], w=[p_])
            if kt >= 4 * Q + 1:
                kb.op("dve", lambda g: g.tensor_tensor(out=p_[:, c0:c0 + 128], in0=p_[:, c0:c0 + 128],
                                                       in1=tri[:], op=ALU.mult), r=[p_, tri], w=[p_])
            rhs = vm[b][:, :] if kt == 0 else vh[b][:, kt, :]
            for j in range(j0, 4):
                kb.op("pe", lambda g: g.matmul(op_[j][:, 0:65], lhsT=p_[0:nk, j * 128:(j + 1) * 128], rhs=rhs,
                                               start=(kt == 0), stop=(kt == 4 * Q + 1 + j)),
                      r=[p_, vh[b], vm[b]], w=[op_[j]])
            if kt == 4 * Q + 4:
                for j in range(4):
                    kb.op("dve", lambda g: g.reciprocal(out=rc[:, 0:1], in_=op_[j][:, 64:65]), r=[op_[j]], w=[rc])
                    kb.op("dve", lambda g: g.tensor_scalar(out=ya[:, 4 * Q + j, h * 64:(h + 1) * 64], in0=op_[j][:, 0:64],
                                                           scalar1=rc[:, 0:1], scalar2=None, op0=ALU.mult),
                          r=[op_[j], rc], pw=[ya])

        emit_s(0)
        emit_s(1)
        for n in range(len(its)):
            if n + 2 < len(its):
                emit_s(n + 2)
            emit_rest(n)
        convert(64)
        kb.dma("sp", D["YA"].t.rearrange("(t p) c -> p t c", p=128), ya[:], r=[ya], w=[D["YA"]])
        kb.barrier()


def phase_c(kb, D, gam):
    with ExitStack() as es:
        sb = lambda n, s, d: kb.sb(es, n, s, d)
        ps = lambda n, s, d: kb.ps(es, n, s, d)
        NB = 2
        rqT = [sb("rqTC%d" % i, [64, LP], BF16) for i in range(NB)]
        rkT = [sb("rkTC%d" % i, [64, LP], BF16) for i in range(NB)]
        rk = [sb("rkC%d" % i, [128, NT, 64], BF16) for i in range(NB)]
        rv = [sb("rvC%d" % i, [128, NT, 128], BF16) for i in range(NB)]
        rg = [sb("rgC%d" % i, [128, NT, 128], BF16) for i in range(NB)]
        yr = [sb("yrC%d" % i, [128, NT, 128], BF16) for i in range(NB)]
        dec = [sb("decC%d" % i, [128, 128], F32) for i in range(NB)]
        xi = [sb("xiC%d" % i, [64, 128], F32) for i in range(NB)]
        zeta = sb("zetaC", [128, 8], F32)
        ggn = sb("ggnC", [128, 1024], F32)
        epsc = sb("epscC", [128, 1], F32)
        S = [sb("SC%d" % i, [64, 128], F32) for i in range(NB)]
        Sb = [sb("SbC%d" % i, [64, 128], BF16) for i in range(NB)]
        sTd = [sb("sTd%d" % i, [128, 128], BF16) for i in range(NB)]
        qxi = [sb("qxi%d" % i, [64, 128], BF16) for i in range(NB)]
        kz = [sb("kz%d" % i, [128, 64], BF16) for i in range(NB)]
        bst = [sb("bst%d" % i, [128, 6], F32) for i in range(NB)]
        mv = [sb("mv%d" % i, [128, 2], F32) for i in range(NB)]
        rsd = [sb("rsd%d" % i, [128, 1], F32) for i in range(NB)]
        nmr = [sb("nmr%d" % i, [128, 1], F32) for i in range(NB)]
        yn = [sb("ynC%d" % i, [128, 128], F32) for i in range(NB)]
        sps = [ps("spsC%d" % i, [128, 512], F32) for i in range(NB)]
        yps = [ps("ypsC%d" % i, [128, 512], F32) for i in range(NB)]
        kvps = [ps("kvpsC%d" % i, [128, 512], F32) for i in range(NB)]
        kb.dma("sp", zeta[:], D["zeta"][:, :], w=[zeta])
        kb.dma("sp", ggn[:], D["g_ret_gn"][:, :], w=[ggn])
        kb.dma("sp", epsc[:], D["epsc"][:, :], w=[epsc])
        steps = [(hp, n, s) for hp in range(4) for n in range(NT) for s in range(2)]

        def slot_loads(hp, s):
            h = 2 * hp + s
            kb.dma("sp", rqT[s][:], D["RQT"][h, :, :], r=[D["RQT"]], w=[rqT[s]])
            kb.dma("sp", rkT[s][:], D["RKT"][h, :, :], r=[D["RKT"]], w=[rkT[s]])
            for t0 in range(0, NT, 11):
                kb.dma("sp", rk[s][:, t0:t0 + 11, :],
                       D["RK"].t.rearrange("(t p) c -> p t c", p=128)[:, t0:t0 + 11, h * 64:(h + 1) * 64],
                       r=[D["RK"]], pw=[rk[s]])
                kb.dma("sp", rv[s][:, t0:t0 + 11, :],
                       D["RV"].t.rearrange("(t p) c -> p t c", p=128)[:, t0:t0 + 11, h * 128:(h + 1) * 128],
                       r=[D["RV"]], pw=[rv[s]])
                kb.dma("sp", rg[s][:, t0:t0 + 11, :],
                       D["RG"].t.rearrange("(t p) c -> p t c", p=128)[:, t0:t0 + 11, h * 128:(h + 1) * 128],
                       r=[D["RG"]], pw=[rg[s]])
            kb.dma("sp", dec[s][:], D["decT"][h, :, :], w=[dec[s]])
            kb.dma("sp", xi[s][:], D["xi"][h, :, :], w=[xi[s]])

        def emit_pre(i):
            hp, n, s = steps[i]
            h = 2 * hp + s
            cs = slice(n * 128, (n + 1) * 128)
            if n == 0:
                slot_loads(hp, s)
            kb.op("pe", lambda g: g.matmul(sps[s][:, 0:128], lhsT=rkT[s][:, cs], rhs=rqT[s][:, cs],
                                           start=True, stop=True), r=[rkT[s], rqT[s]], w=[sps[s]])
            if n > 0:
                kb.op("pool", lambda g: g.tensor_tensor(out=qxi[s][:], in0=rqT[s][:, cs], in1=xi[s][:],
                                                        op=ALU.mult), r=[rqT[s], xi[s]], w=[qxi[s]])
            if n < NT - 1:
                kb.op("pool", lambda g: g.tensor_scalar(out=kz[s][:], in0=rk[s][:, n, :], scalar1=zeta[:, h:h + 1],
                                                        scalar2=None, op0=ALU.mult), r=[rk[s], zeta], w=[kz[s]])

        def emit_rest(i):
            hp, n, s = steps[i]
            h = 2 * hp + s
            cs = slice(n * 128, (n + 1) * 128)
            if n == NT - 1:
                kb_out = True
            kb.op("dve", lambda g: g.tensor_tensor(out=sTd[s][:], in0=sps[s][:, 0:128], in1=dec[s][:],
                                                   op=ALU.mult), r=[sps[s], dec[s]], w=[sTd[s]])
            kb.op("pe", lambda g: g.matmul(yps[s][:, 0:128], lhsT=sTd[s][:], rhs=rv[s][:, n, :],
                                           start=True, stop=(n == 0)), r=[sTd[s], rv[s]], w=[yps[s]])
            if n > 0:
                kb.op("pe", lambda g: g.matmul(yps[s][:, 0:128], lhsT=qxi[s][:], rhs=Sb[s][:],
                                               start=False, stop=True), r=[qxi[s], Sb[s]], w=[yps[s]])
            if n < NT - 1:
                kb.op("pe", lambda g: g.matmul(kvps[s][0:64, 0:128], lhsT=kz[s][:], rhs=rv[s][:, n, :],
                                               start=True, stop=True), r=[kz[s], rv[s]], w=[kvps[s]])
                if n == 0:
                    kb.op("dve", lambda g: g.tensor_copy(out=S[s][:], in_=kvps[s][0:64, 0:128]),
                          r=[kvps[s]], w=[S[s]])
                else:
                    kb.op("dve", lambda g: g.scalar_tensor_tensor(out=S[s][:], in0=S[s][:], scalar=float(gam[h]),
                                                                  in1=kvps[s][0:64, 0:128], op0=ALU.mult,
                                                                  op1=ALU.add), r=[S[s], kvps[s]], w=[S[s]])
                kb.op("act", lambda g: g.copy(out=Sb[s][:], in_=S[s][:]), r=[S[s]], w=[Sb[s]])
            if n == 0:
                return
            kb.op("dve", lambda g: g.bn_stats(out=bst[s][:], in_=yps[s][:, 0:128]), r=[yps[s]], w=[bst[s]])
            kb.op("dve", lambda g: g.bn_aggr(out=mv[s][:], in_=bst[s][:]), r=[bst[s]], w=[mv[s]])
            kb.op("act", lambda g: g.activation(out=rsd[s][:], in_=mv[s][:, 1:2], func=AF.Sqrt,
                                                bias=epsc[:, 0:1], scale=1.0), r=[mv[s], epsc], w=[rsd[s]])
            kb.op("dve", lambda g: g.reciprocal(out=rsd[s][:], in_=rsd[s][:]), r=[rsd[s]], w=[rsd[s]])
            kb.op("dve", lambda g: g.scalar_tensor_tensor(out=nmr[s][:], in0=mv[s][:, 0:1], scalar=-1.0,
                                                          in1=rsd[s][:], op0=ALU.mult, op1=ALU.mult),
                  r=[mv[s], rsd[s]], w=[nmr[s]])
            kb.op("act", lambda g: g.activation(out=yn[s][:], in_=yps[s][:, 0:128], func=AF.Identity,
                                                bias=nmr[s][:, 0:1], scale=rsd[s][:, 0:1]),
                  r=[yps[s], nmr[s], rsd[s]], w=[yn[s]])
            kb.op("pool", lambda g: g.tensor_tensor(out=yn[s][:], in0=yn[s][:], in1=ggn[:, h * 128:(h + 1) * 128],
                                                    op=ALU.mult), r=[yn[s], ggn], w=[yn[s]])
            kb.op("pool", lambda g: g.tensor_tensor(out=yr[s][:, n, :], in0=yn[s][:], in1=rg[s][:, n, :],
                                                    op=ALU.mult), r=[yn[s], rg[s]], pw=[yr[s]])

        def emit_out(i):
            hp, n, s = steps[i]
            h = 2 * hp + s
            if n == NT - 1:
                kb.dma("sp", D["YR"].t.rearrange("(t p) c -> p t c", p=128)[:, 1:NT, h * 128:(h + 1) * 128],
                       yr[s][:, 1:NT, :], r=[yr[s]], pw=[D["YR"]])

        emit_pre(0)
        for i in range(len(steps)):
            if i + 1 < len(steps):
                emit_pre(i + 1)
            emit_rest(i)
            emit_out(i)
        kb.barrier()


def phase_d(kb, D, tiles, stop=0):
    with ExitStack() as es:
        sb = lambda n, s, d: kb.sb(es, n, s, d)
        ps = lambda n, s, d: kb.ps(es, n, s, d)
        w_mo = sb("w_mo", [128, 4, 1024], BF16)
        w_ro = sb("w_ro", [128, 8, 1024], BF16)
        w_xo = sb("w_xo", [128, 8, 1024], BF16)
        w_pq = sb("w_pq", [128, 8, 1024], BF16)
        keys = sb("keysD", [128, 256], BF16)
        ident = sb("identD", [128, 128], BF16)
        zc = sb("zcD", [128, 255], F32)
        gffn = sb("gffn", [128, 1024], F32)
        epsc = sb("epscD", [128, 1], F32)
        xt = sb("xtD", [128, 1024], F32)
        ya = sb("yaD", [128, 512], BF16)
        yr = sb("yrD", [128, 1024], BF16)
        za = sb("zaD", [128, 1024], BF16)
        zr = sb("zrD", [128, 1024], BF16)
        yaT = sb("yaTD", [128, 512], BF16)
        yrT = sb("yrTD", [128, 1024], BF16)
        m1 = sb("m1D", [128, 1024], F32)
        m2 = sb("m2D", [128, 1024], F32)
        mg = sb("mgD", [128, 1024], BF16)
        mgT = sb("mgTD", [128, 1024], BF16)
        hh2 = [sb("hhD%d" % i, [128, 1024], F32) for i in range(2)]
        xn2 = [sb("xnD%d" % i, [128, 1024], BF16) for i in range(2)]
        eT2 = [sb("eTD%d" % i, [128, 128], I32) for i in range(2)]
        gT2 = [sb("gTD%d" % i, [128, 128], F32) for i in range(2)]
        junk = sb("junkD", [128, 1024], BF16)
        junkr = sb("junkrD", [128, 1024], BF16)
        ss = sb("ssD", [128, 1], F32)
        rs = sb("rsD", [128, 1], F32)
        xnT = sb("xnTD", [128, 1024], BF16)
        pqT = sb("pqTD", [128, 1024], BF16)
        sc = sb("scD", [128, 8, 256], F32)
        scm = sb("scmD", [128, 8, 256], F32)
        v12 = sb("v12D", [128, 8, 2, 16], F32)
        i12 = sb("i12D", [128, 8, 2, 16], U32)
        i12f = sb("i12fD", [128, 8, 2, 16], F32)
        cand = sb("candD", [128, 8, 256], F32)
        candm = sb("candmD", [128, 8, 256], F32)
        cv = sb("cvD", [128, 8, 16], F32)
        ci = sb("ciD", [128, 8, 16], U32)
        oh = sb("ohD", [128, 8, 16, 16], F32)
        af = sb("afD", [128, 8, 16], F32)
        bf = sb("bfD", [128, 8, 16], F32)
        iota = sb("iotaD", [128, 32], F32)
        e1b = sb("e1bD", [128, 128], BF16)
        e2b = sb("e2bD", [128, 128], BF16)
        ghi = sb("ghiD", [128, 128], BF16)
        glo = sb("gloD", [128, 128], BF16)
        tsb = sb("tsbD", [128, 512], BF16)
        cf = sb("cfD", [128, 8, 16], F32)
        ef = sb("efD", [128, 128], F32)
        es_ = sb("esD", [128, 8, 16], F32)
        sm8 = sb("sm8D", [128, 8], F32)
        gts = sb("gtsD", [128, 128], F32)
        NG = 8
        uvs = [sb("uvs%d" % i, [128, 2048], BF16) for i in range(NG)]
        acol = [sb("acol%d" % i, [128, 1], F32) for i in range(NG)]
        wcol = [sb("wcol%d" % i, [128, 1], F32) for i in range(NG)]
        selb = [sb("selD%d" % i, [128, 128], BF16) for i in range(NG)]
        G = [sb("GD%d" % i, [128, 128], BF16) for i in range(NG)]
        ot = sb("otD", [128, 1024], F32)
        pA = ps("pA", [128, 1024], F32)
        pB = ps("pB", [128, 1024], F32)
        pC = ps("pC", [128, 1024], F32)
        pT = ps("pTD", [128, 1024], BF16)
        pF = ps("pFD", [128, 512], F32)

        for k in range(4):
            kb.dma("pool", w_mo[:, k, :], D["w_mla_out"][k * 128:(k + 1) * 128, :], pw=[w_mo])
        for t_, n_ in ((w_ro, "w_ret_out"), (w_xo, "w_mix_out"), (w_pq, "w_peer_q")):
            for k in range(8):
                kb.dma("pool", t_[:, k, :], D[n_][k * 128:(k + 1) * 128, :], pw=[t_])
        kb.dma("pool", keys[:], D["keysbd"][:, :], w=[keys])
        kb.dma("pool", ident[:], D["ident"][:, :], w=[ident])
        kb.dma("sp", zc[:], D["zc"][:, :], w=[zc])
        kb.dma("sp", iota[:], D["iota16"][:, :], w=[iota])
        kb.dma("sp", gffn[:], D["g_ffn"][:, :], w=[gffn])
        kb.dma("sp", epsc[:], D["epsc"][:, :], w=[epsc])

        def route(T):
            r0 = T * 128
            hh, xn, eT, gT = hh2[T % 2], xn2[T % 2], eT2[T % 2], gT2[T % 2]
            kb.dma("sp", xt[:], D["xpad"][r0:r0 + 128, :], w=[xt])
            kb.dma("sp", ya[:], D["YA"][r0 - 128:r0, :], r=[D["YA"]], w=[ya])
            kb.dma("sp", yr[:], D["YR"][r0:r0 + 128, :], r=[D["YR"]], w=[yr])
            kb.dma("sp", za[:], D["ZA"][r0:r0 + 128, :], r=[D["ZA"]], w=[za])
            kb.dma("sp", zr[:], D["ZR"][r0:r0 + 128, :], r=[D["ZR"]], w=[zr])
            yield

            def transposes(src, dst, nk):
                for k in range(nk):
                    kb.op("pe", lambda g: g.transpose(out=pT[:, k * 128:(k + 1) * 128], in_=src[:, k * 128:(k + 1) * 128],
                                                      identity=ident[:]), r=[src, ident], w=[pT])
                    if k % 4 == 3:
                        yield
                kb.op("act", lambda g: g.copy(out=dst[:, 0:nk * 128], in_=pT[:, 0:nk * 128]), r=[pT], w=[dst])
                yield

            def proj(srcT, w, nk, evac):
                for a0 in (0, 512):
                    for k in range(nk):
                        kb.op("pe", lambda g: g.matmul(pF[:, 0:512], lhsT=srcT[:, k * 128:(k + 1) * 128],
                                                       rhs=w[:, k, a0:a0 + 512], start=(k == 0), stop=(k == nk - 1)),
                              r=[srcT, w], w=[pF])
                        if k % 4 == 3:
                            yield
                    evac(a0)
                    yield

            yield from transposes(ya, yaT, 4)
            yield from proj(yaT, w_mo, 4, lambda a0: kb.op("dve", lambda g: g.tensor_tensor(
                out=m1[:, a0:a0 + 512], in0=pF[:, 0:512], in1=za[:, a0:a0 + 512], op=ALU.mult), r=[pF, za], pw=[m1]))
            yield from transposes(yr, yrT, 8)
            yield from proj(yrT, w_ro, 8, lambda a0: kb.op("dve", lambda g: g.tensor_tensor(
                out=m2[:, a0:a0 + 512], in0=pF[:, 0:512], in1=zr[:, a0:a0 + 512], op=ALU.mult), r=[pF, zr], pw=[m2]))
            kb.op("pool", lambda g: g.tensor_tensor(out=mg[:], in0=m1[:], in1=m2[:], op=ALU.add), r=[m1, m2], w=[mg])
            yield
            yield from transposes(mg, mgT, 8)
            yield from proj(mgT, w_xo, 8, lambda a0: kb.op("dve", lambda g: g.tensor_tensor(
                out=hh[:, a0:a0 + 512], in0=pF[:, 0:512], in1=xt[:, a0:a0 + 512], op=ALU.add), r=[pF, xt], pw=[hh]))
            kb.op("act", lambda g: g.activation(out=junkr[:], in_=hh[:], func=AF.Square, accum_out=ss[:, 0:1]),
                  r=[hh], w=[junkr, ss])
            rstd_from_ss(kb, (ss[:, 0:1], ss), (rs[:, 0:1], rs), epsc, 1024.0)
            kb.op("dve", lambda g: g.scalar_tensor_tensor(out=xn[:], in0=hh[:], scalar=rs[:, 0:1], in1=gffn[:],
                                                          op0=ALU.mult, op1=ALU.mult), r=[hh, rs, gffn], w=[xn])
            yield
            yield from transposes(xn, xnT, 8)
            for hb in range(2):
                for h in range(4 * hb, 4 * hb + 4):
                    for k in range(8):
                        kb.op("pe", lambda g: g.matmul(pF[:, (h % 4) * 128:(h % 4 + 1) * 128],
                                                       lhsT=w_pq[:, k, h * 128:(h + 1) * 128],
                                                       rhs=xnT[:, k * 128:(k + 1) * 128], start=(k == 0), stop=(k == 7)),
                              r=[w_pq, xnT], w=[pF])
                        if k % 4 == 3:
                            yield
                kb.op("act", lambda g: g.copy(out=pqT[:, hb * 512:(hb + 1) * 512], in_=pF[:, 0:512]), r=[pF], pw=[pqT])
                yield
            for hp in range(4):
                for h in (2 * hp, 2 * hp + 1):
                    kb.op("pe", lambda g: g.matmul(pF[:, (h % 2) * 256:(h % 2 + 1) * 256], lhsT=pqT[:, h * 128:(h + 1) * 128],
                                                   rhs=keys[:], start=True, stop=True), r=[pqT, keys], w=[pF])
                kb.op("act", lambda g: g.copy(out=sc[:, 2 * hp:2 * hp + 2, :].rearrange("p h k -> p (h k)"), in_=pF[:, 0:512]),
                      r=[pF], pw=[sc])
                yield
            for h in range(8):
                for hf in range(2):
                    src = sc[:, h, hf * 128:(hf + 1) * 128]
                    srm = scm[:, h, hf * 128:(hf + 1) * 128]
                    kb.op("dve", lambda g: g.max(out=v12[:, h, hf, 0:8], in_=src), r=[sc], pw=[v12])
                    kb.op("dve", lambda g: g.max_index(out=i12[:, h, hf, 0:8], in_max=v12[:, h, hf, 0:8], in_values=src),
                          r=[sc, v12], pw=[i12])
                    kb.op("dve", lambda g: g.match_replace(out=srm, in_to_replace=v12[:, h, hf, 0:8], in_values=src,
                                                           imm_value=NEG), r=[sc, v12], pw=[scm])
                    yield
                    kb.op("dve", lambda g: g.max(out=v12[:, h, hf, 8:16], in_=srm), r=[scm], pw=[v12])
                    kb.op("dve", lambda g: g.max_index(out=i12[:, h, hf, 8:16], in_max=v12[:, h, hf, 8:16],
                                                       in_values=srm), r=[scm, v12], pw=[i12])
                    yield
            kb.op("dve", lambda g: g.tensor_tensor(out=cand[:].rearrange("p h (a b) -> p h a b", a=16),
                                                   in0=v12[:, :, 0, :].unsqueeze(3).to_broadcast([128, 8, 16, 16]),
                                                   in1=v12[:, :, 1, :].unsqueeze(2).to_broadcast([128, 8, 16, 16]),
                                                   op=ALU.add), r=[v12], w=[cand])
            yield
            for h in range(8):
                kb.op("dve", lambda g: g.max(out=cv[:, h, 0:8], in_=cand[:, h, :]), r=[cand], pw=[cv])
                kb.op("dve", lambda g: g.max_index(out=ci[:, h, 0:8], in_max=cv[:, h, 0:8], in_values=cand[:, h, :]),
                      r=[cand, cv], pw=[ci])
                kb.op("dve", lambda g: g.match_replace(out=candm[:, h, :], in_to_replace=cv[:, h, 0:8],
                                                       in_values=cand[:, h, :], imm_value=NEG), r=[cand, cv], pw=[candm])
                yield
                kb.op("dve", lambda g: g.max(out=cv[:, h, 8:16], in_=candm[:, h, :]), r=[candm], pw=[cv])
                kb.op("dve", lambda g: g.max_index(out=ci[:, h, 8:16], in_max=cv[:, h, 8:16], in_values=candm[:, h, :]),
                      r=[candm, cv], pw=[ci])
                yield
            kb.op("dve", lambda g: g.tensor_tensor(out=es_[:], in0=cv[:],
                                                   in1=cv[:, :, 0:1].to_broadcast([128, 8, 16]), op=ALU.subtract),
                  r=[cv], w=[es_])
            kb.op("act", lambda g: g.activation(out=es_[:], in_=es_[:], func=AF.Exp), r=[es_], w=[es_])
            yield
            kb.op("dve", lambda g: g.tensor_reduce(out=sm8[:], in_=es_[:], axis=AX.X, op=ALU.add), r=[es_], w=[sm8])
            kb.op("dve", lambda g: g.reciprocal(out=sm8[:], in_=sm8[:]), r=[sm8], w=[sm8])
            kb.op("dve", lambda g: g.tensor_tensor(out=gts[:].rearrange("p (h k) -> p h k", h=8), in0=es_[:],
                                                   in1=sm8[:].unsqueeze(2).to_broadcast([128, 8, 16]), op=ALU.mult),
                  r=[es_, sm8], w=[gts])
            yield
            kb.op("dve", lambda g: g.tensor_copy(out=cf[:], in_=ci[:]), r=[ci], w=[cf])
            kb.op("dve", lambda g: g.tensor_copy(out=i12f[:], in_=i12[:]), r=[i12], w=[i12f])
            yield
            kb.op("dve", lambda g: g.tensor_tensor(out=oh[:], in0=cf[:].unsqueeze(3).to_broadcast([128, 8, 16, 16]),
                                                   in1=iota[:, 16:32].unsqueeze(1).unsqueeze(1).to_broadcast([128, 8, 16, 16]),
                                                   op=ALU.is_ge), r=[cf, iota], w=[oh])
            yield
            kb.op("dve", lambda g: g.tensor_reduce(out=af[:], in_=oh[:], axis=AX.X, op=ALU.add), r=[oh], w=[af])
            kb.op("dve", lambda g: g.scalar_tensor_tensor(out=bf[:], in0=af[:], scalar=-16.0, in1=cf[:], op0=ALU.mult,
                                                          op1=ALU.add), r=[af, cf], w=[bf])
            yield
            for (sel_, hf, dst) in ((af, 0, e1b), (bf, 1, e2b)):
                kb.op("dve", lambda g: g.tensor_tensor(out=oh[:], in0=sel_[:].unsqueeze(3).to_broadcast([128, 8, 16, 16]),
                                                       in1=iota[:, 0:16].unsqueeze(1).unsqueeze(1).to_broadcast([128, 8, 16, 16]),
                                                       op=ALU.is_equal), r=[sel_, iota], w=[oh])
                yield
                kb.op("dve", lambda g: g.tensor_tensor(out=oh[:], in0=oh[:],
                                                       in1=i12f[:, :, hf, :].unsqueeze(2).to_broadcast([128, 8, 16, 16]),
                                                       op=ALU.mult), r=[oh, i12f], w=[oh])
                yield
                with kb.nc.allow_low_precision("one-hot select of an integer < 128: exact in bf16"):
                    kb.op("dve", lambda g: g.tensor_reduce(out=dst[:].rearrange("p (h k) -> p h k", h=8), in_=oh[:],
                                                           axis=AX.X, op=ALU.add), r=[oh], w=[dst])
                yield
            kb.op("dve", lambda g: g.tensor_copy(out=ghi[:], in_=gts[:]), r=[gts], w=[ghi])
            kb.op("dve", lambda g: g.tensor_tensor(out=glo[:], in0=gts[:], in1=ghi[:], op=ALU.subtract), r=[gts, ghi], w=[glo])
            for i_, src_ in enumerate((e1b, e2b, ghi, glo)):
                kb.op("pe", lambda g: g.transpose(out=pT[:, i_ * 128:(i_ + 1) * 128], in_=src_[:], identity=ident[:]),
                      r=[src_, ident], w=[pT])
            kb.op("act", lambda g: g.copy(out=tsb[:], in_=pT[:, 0:512]), r=[pT], w=[tsb])
            yield
            kb.op("dve", lambda g: g.scalar_tensor_tensor(out=ef[:], in0=tsb[:, 0:128], scalar=128.0, in1=tsb[:, 128:256],
                                                          op0=ALU.mult, op1=ALU.add), r=[tsb], w=[ef])
            kb.op("dve", lambda g: g.tensor_copy(out=eT[:], in_=ef[:]), r=[ef], w=[eT])
            kb.op("dve", lambda g: g.tensor_tensor(out=gT[:], in0=tsb[:, 256:384], in1=tsb[:, 384:512], op=ALU.add),
                  r=[tsb], w=[gT])
            yield

        def experts(T, nxt):
            r0 = T * 128
            hh, xn, eT, gT = hh2[T % 2], xn2[T % 2], eT2[T % 2], gT2[T % 2]
            pBB = [pB, pA]

            def s_sel(t):
                sl = selb[t % NG]
                kb.op("act", lambda g: g.copy(out=sl[:], in_=ident[:, t:t + 1].to_broadcast([128, 128])),
                      r=[ident], w=[sl])

            def s0(t):
                uv = uvs[t % NG]
                kb.dma("pool", None, None, r=[eT, D["UVB"]], w=[uv],
                       fn=lambda g: g.indirect_dma_start(out=uv[:], out_offset=None, in_=D["UVB"][:, :],
                                                         in_offset=bass.IndirectOffsetOnAxis(ap=eT[:, t:t + 1], axis=0)))

            def s0b(t):
                sl = selb[t % NG]
                pb = pBB[t % 2]
                for (a0, a1) in ((0, 512), (512, 1024)):
                    kb.op("pe", lambda g: g.matmul(pb[:, a0:a1], lhsT=sl[:], rhs=xn[:, a0:a1], start=True, stop=True),
                          r=[sl, xn], w=[pb])

            def s1(t):
                uv = uvs[t % NG]
                pb = pBB[t % 2]
                ac = acol[t % NG]
                wc = wcol[t % NG]
                kb.op("dve", lambda g: g.scalar_tensor_tensor(out=junk[:], in0=uv[:, 0:1024], scalar=1.0, in1=pb[:],
                                                              op0=ALU.mult, op1=ALU.mult, accum_out=ac[:, 0:1]),
                      r=[uv, pb], w=[junk, ac])
                kb.op("act", lambda g: g.activation(out=wc[:, 0:1], in_=ac[:, 0:1], func=AF.Gelu), r=[ac], w=[wc])

            def s2(t):
                uv = uvs[t % NG]
                g_ = G[t % NG]
                wc = wcol[t % NG]
                kb.op("dve", lambda g: g.tensor_scalar(out=g_[:], in0=zc[:, 127 - t:255 - t], scalar1=wc[:, 0:1],
                                                       scalar2=gT[:, t:t + 1], op0=ALU.mult, op1=ALU.mult),
                      r=[zc, wc, gT], w=[g_])
                for (a0, a1) in ((0, 512), (512, 1024)):
                    kb.op("pe", lambda g: g.matmul(pC[:, a0:a1], lhsT=g_[:], rhs=uv[:, 1024 + a0:1024 + a1], start=(t == 0),
                                                   stop=(t == 127)), r=[g_, uv], w=[pC])

            for j in range(4):
                s_sel(j)
            for i in range(128 + 5):
                if i + 4 < 128:
                    s_sel(i + 4)
                if i < 128:
                    s0(i)
                if 0 <= i - 2 < 128:
                    s0b(i - 2)
                if 0 <= i - 3 < 128:
                    s1(i - 3)
                if nxt is not None:
                    next(nxt, None)
                    next(nxt, None)
                if 0 <= i - 5 < 128:
                    s2(i - 5)
            kb.op("dve", lambda g: g.tensor_tensor(out=ot[:], in0=pC[:], in1=hh[:], op=ALU.add), r=[pC, hh], w=[ot])
            kb.dma("sp", D["out"][r0 - 128:r0, :], ot[:], r=[ot], pw=[D["out"]])

        tiles = list(tiles)
        for _ in route(tiles[0]):
            pass
        for i, T in enumerate(tiles):
            nxt = route(tiles[i + 1]) if i + 1 < len(tiles) else None
            experts(T, nxt)
            if nxt is not None:
                for _ in nxt:
                    pass
        kb.barrier()


def host_constants():
    c = {}
    pos = (np.arange(LP) - 112).astype(np.float32)

    def tab(half):
        inv = (10000.0 ** (-np.arange(half, dtype=np.float32) / half)).astype(np.float32)
        ang = (pos[:, None] * inv[None, :]).astype(np.float32)
        lay = lambda a: np.ascontiguousarray(a.reshape(NT, 128, half).transpose(1, 0, 2).reshape(128, NT * half))
        return lay(np.cos(ang).astype(np.float32)), lay(np.sin(ang).astype(np.float32))

    c["cosm"], c["sinm"] = tab(16)
    c["cosr"], c["sinr"] = tab(32)
    H, C = 8, 128
    lg = np.log(1.0 - 2.0 ** (-5.0 - np.arange(H, dtype=np.float32))).astype(np.float32)
    idx = np.arange(C, dtype=np.float32)
    diff = idx[:, None] - idx[None, :]
    decay = np.where(diff[None] >= 0, np.exp(np.maximum(diff, 0.0)[None] * lg[:, None, None]), 0.0)
    c["decT"] = np.ascontiguousarray(decay.transpose(0, 2, 1) * 0.125).astype(np.float32)
    zeta = np.exp((C - 1 - idx)[None, :] * lg[:, None]) * 0.125
    c["zeta"] = np.ascontiguousarray(zeta.T).astype(np.float32)
    xi = np.exp((idx + 1)[None, :] * lg[:, None])
    c["xi"] = np.ascontiguousarray(np.broadcast_to(xi[:, None, :], (H, 64, C))).astype(np.float32)
    c["gamma"] = np.exp(C * lg).astype(np.float32)
    c["ident"] = np.eye(128, dtype=np.float32)
    k = np.arange(128)
    c["tri"] = (k[:, None] <= k[None, :]).astype(np.float32)
    zc = np.zeros((128, 255), np.float32)
    zc[:, 127] = 1.0
    c["zc"] = zc
    io = np.concatenate([np.arange(16, dtype=np.float32), 16.0 * np.arange(1, 17, dtype=np.float32)])
    c["iota16"] = np.ascontiguousarray(np.broadcast_to(io[None], (128, 32)))
    c["epsc"] = np.full((128, 1), EPS, np.float32)
    return c


SCRATCH = {"QT": ([8, 96, LP], BF16), "KT": ([8, 96, LP], BF16), "VA": ([LP, 520], BF16),
           "RQT": ([8, 64, LP], BF16), "RKT": ([8, 64, LP], BF16), "RK": ([LP, 512], BF16),
           "RV": ([LP, 1024], BF16), "RG": ([LP, 1024], BF16), "ZA": ([LP, 1024], BF16), "ZR": ([LP, 1024], BF16),
           "YA": ([4096, 512], BF16), "YR": ([LP, 1024], BF16),
           "UVB": ([16384, 2048], BF16)}

INPUTS = {"xpad": [LP, 1024], "w_in": [1024, 5792], "w_uq": [384, 768], "w_ukv": [256, 1024],
          "w_mla_out": [512, 1024], "w_ret_out": [1024, 1024], "w_mix_out": [1024, 1024], "w_peer_q": [1024, 1024],
          "peer_u": [16384, 1024], "peer_v": [16384, 1024], "keysbd": [128, 256],
          "g_mix": [128, 1024], "g_q_lora": [128, 384], "g_kv_lora": [128, 256], "g_qk_q": [128, 96],
          "g_qk_k": [128, 96], "g_ret_gn": [128, 1024], "g_ffn": [128, 1024],
          "cosm": [128, NT * 16], "sinm": [128, NT * 16], "cosr": [128, NT * 32], "sinr": [128, NT * 32],
          "decT": [8, 128, 128], "zeta": [128, 8], "xi": [8, 64, 128], "ident": [128, 128], "tri": [128, 128],
          "zc": [128, 255], "iota16": [128, 32], "epsc": [128, 1]}


def build(phases="abcd", debug=False, tiles=None, ext_in=(), stop=0):
    kb = KB()
    D = {}
    for n, shp in INPUTS.items():
        D[n] = kb.dram(n, shp, F32, "ExternalInput")
    for n, (shp, dt) in SCRATCH.items():
        D[n] = kb.dram(n, shp, dt, "ExternalInput" if n in ext_in else ("ExternalOutput" if debug else "Internal"))
    D["out"] = kb.dram("out", [4096, 1024], F32, "ExternalOutput")
    if debug:
        D["DBG"] = kb.dram("DBG", [128, 512], F32, "ExternalOutput")
    gam = host_constants()["gamma"]
    if "a" in phases:
        phase_a(kb, D)
    if "b" in phases:
        phase_b(kb, D)
    if "c" in phases:
        phase_c(kb, D, gam)
    if "d" in phases:
        phase_d(kb, D, tiles if tiles is not None else list(range(1, NT)), stop)
    kb.barrier()
    return kb


def make_in_maps(inputs):
    c = host_constants()
    f = lambda a: np.ascontiguousarray(np.asarray(a, dtype=np.float32))
    rep = lambda v: np.ascontiguousarray(np.broadcast_to(f(v).reshape(1, -1), (128, f(v).size)))
    shared = {
        "w_in": f(inputs["w_in"][0]), "w_uq": f(inputs["w_uq"][0]), "w_ukv": f(inputs["w_ukv"][0]),
        "w_mla_out": f(inputs["w_mla_out"][0]), "w_ret_out": f(inputs["w_ret_out"][0]),
        "w_mix_out": f(inputs["w_mix_out"][0]), "w_peer_q": f(inputs["w_peer_q"][0]),
        "peer_u": f(inputs["peer_u"][0]), "peer_v": f(inputs["peer_v"][0]),
        "g_mix": rep(inputs["g_mix"][0]), "g_q_lora": rep(inputs["g_q_lora"][0]),
        "g_kv_lora": rep(inputs["g_kv_lora"][0]), "g_qk_q": rep(inputs["g_qk_q"][0]),
        "g_qk_k": rep(inputs["g_qk_k"][0]), "g_ret_gn": rep(inputs["g_ret_gn"][0]), "g_ffn": rep(inputs["g_ffn"][0]),
    }
    kbd = np.zeros((128, 256), np.float32)
    kbd[0:64, 0:128] = f(inputs["peer_keys_1"][0]).T
    kbd[64:128, 128:256] = f(inputs["peer_keys_2"][0]).T
    shared["keysbd"] = kbd
    for n in ("cosm", "sinm", "cosr", "sinr", "decT", "zeta", "xi", "ident", "tri", "zc", "iota16", "epsc"):
        shared[n] = c[n]
    x = f(inputs["x"])
    meta = f(inputs["meta_tokens"])
    maps = []
    for b in range(x.shape[0]):
        xp = np.zeros((LP, 1024), np.float32)
        xp[112:128] = meta
        xp[128:] = x[b]
        m = dict(shared)
        m["xpad"] = xp
        maps.append(m)
    return maps


def kernel(**inputs):
    kb = build("abcd")
    maps = make_in_maps(inputs)
    res = run_bass_kernel_spmd(kb.nc, maps, core_ids=list(range(8)))
    return np.stack([np.asarray(r["out"], dtype=np.float32) for r in res.results], axis=0)
```
